# Optimizing a Trainium2 kernel written in Bass

```python
import jax, jax.numpy as jnp
from jax import lax
import numpy as np

D_MODEL = 1024
BATCH = 8
SEQ = 2048
DEPTH = 4

GRID_W = 64
CTX_LEN = 256
N_MIXERS = 2
N_CONV_LAYERS = (DEPTH + 1) // 2
N_REC_LAYERS = DEPTH // 2
CONV_WIDTH = 31
REC_HEADS = 8
REC_HEAD_DIM = D_MODEL // REC_HEADS
REC_CHUNK = 16
D_FF = ((8 * D_MODEL // 3 + 127) // 128) * 128
N_EXPERTS = 8
TOP_K = 2
EPS = 1e-6

kernel_name = "hybrid_conv_hgrn2_moe_dit_prefix"


def rms_norm(x, g):
    xf = x.astype(jnp.float32)
    y = xf * lax.rsqrt(jnp.mean(xf * xf, axis=-1, keepdims=True) + EPS)
    return (y * g.astype(jnp.float32)).astype(x.dtype)


def layer_norm(x, g, b):
    xf = x.astype(jnp.float32)
    mu = jnp.mean(xf, axis=-1, keepdims=True)
    var = jnp.mean(jnp.square(xf - mu), axis=-1, keepdims=True)
    y = (xf - mu) * lax.rsqrt(var + EPS)
    return (y * g.astype(jnp.float32) + b.astype(jnp.float32)).astype(x.dtype)


def modulate(h, shift, scale):
    return h * (1.0 + scale) + shift


def swiglu(h, w1, w3, w2):
    return (jax.nn.silu(h @ w1) * (h @ w3)) @ w2


def moe_swiglu(h, router, w1, w3, w2):
    logits = jnp.einsum('bld,de->ble', h, router).astype(jnp.float32)
    top_v, top_i = lax.top_k(logits, TOP_K)
    top_w = jax.nn.softmax(top_v, axis=-1)
    gates = jnp.sum(jax.nn.one_hot(top_i, N_EXPERTS, dtype=jnp.float32) * top_w[..., None], axis=-2)
    gates = gates.astype(h.dtype)
    out = jnp.zeros_like(h)
    for e in range(N_EXPERTS):
        out = out + gates[..., e:e + 1] * swiglu(h, w1[e], w3[e], w2[e])
    return out


def dwconv1d(u, w, b):
    pad = (w.shape[0] - 1) // 2
    y = lax.conv_general_dilated(u, w[:, None, :].astype(u.dtype), window_strides=(1,),
                                 padding=[(pad, pad)], dimension_numbers=('NWC', 'WIO', 'NWC'),
                                 feature_group_count=u.shape[-1])
    return y + b


def axial_dwconv(u, w, b):
    bsz, s, d = u.shape
    rows = s // GRID_W
    half = d // 2
    g = u.reshape(bsz, rows, GRID_W, d)
    along_w = dwconv1d(g[..., :half].reshape(bsz * rows, GRID_W, half), w[:, :half], b[:half])
    along_w = along_w.reshape(bsz, rows, GRID_W, half)
    cols = jnp.swapaxes(g[..., half:], 1, 2).reshape(bsz * GRID_W, rows, d - half)
    along_h = dwconv1d(cols, w[:, half:], b[half:]).reshape(bsz, GRID_W, rows, d - half)
    along_h = jnp.swapaxes(along_h, 1, 2)
    return jnp.concatenate([along_w, along_h], axis=-1).reshape(bsz, s, d)


def conv_module(h, dwconv, pw1_w, pw1_b, dw_w, dw_b, ln_g, ln_b, pw2_w, pw2_b):
    a = h @ pw1_w + pw1_b
    d = a.shape[-1] // 2
    u = a[..., :d] * jax.nn.sigmoid(a[..., d:])
    u = dwconv(u, dw_w, dw_b)
    u = jax.nn.silu(layer_norm(u, ln_g, ln_b))
    return u @ pw2_w + pw2_b


def _heads(t):
    return t.reshape(t.shape[0], t.shape[1], REC_HEADS, REC_HEAD_DIM).astype(jnp.float32)


def _forget(z, lb):
    f = lb + (1.0 - lb) * jax.nn.sigmoid(_heads(z))
    return 1.0 - f, jnp.log(f)


def gla_chunked(q, k, v, logf, s0):
    bsz, length, nh, dk = q.shape
    dv = v.shape[-1]
    n = length // REC_CHUNK
    c = REC_CHUNK
    rs = lambda t: t.reshape(bsz, n, c, nh, t.shape[-1])
    q, k, v, logf = rs(q), rs(k), rs(v), rs(logf)
    bcum = jnp.cumsum(logf, axis=2)
    b_last = bcum[:, :, -1:]
    q_in = q * jnp.exp(bcum)
    k_in = k * jnp.exp(-bcum)
    k_st = k * jnp.exp(b_last - bcum)
    mask = jnp.tril(jnp.ones((c, c), dtype=bool))
    att = jnp.where(mask, jnp.einsum('bnthd,bnshd->bnhts', q_in, k_in), 0.0)
    o_intra = jnp.einsum('bnhts,bnshe->bnthe', att, v)

    def step(s, inp):
        q_j, k_j, v_j, dec_j = inp
        o_j = jnp.einsum('bthd,bhde->bthe', q_j, s)
        s = dec_j[..., None] * s + jnp.einsum('bshd,bshe->bhde', k_j, v_j)
        return s, o_j

    xs = (jnp.moveaxis(q_in, 1, 0), jnp.moveaxis(k_st, 1, 0), jnp.moveaxis(v, 1, 0),
          jnp.moveaxis(jnp.exp(b_last[:, :, 0]), 1, 0))
    s_final, o_inter = lax.scan(step, s0, xs)
    o = o_intra + jnp.moveaxis(o_inter, 0, 1)
    return o.reshape(bsz, length, nh, dv), s_final


def gla_final_state(k, v, logf):
    bcum = jnp.cumsum(logf, axis=1)
    return jnp.einsum('blhd,blhe->bhde', k * jnp.exp(bcum[:, -1:] - bcum), v)


def hgrn2_mixer(h_lat, h_ctx, w_in, lb_fwd, lb_bwd, onorm_g, w_o, ctx_out):
    d = h_lat.shape[-1]
    lbf = lb_fwd.reshape(REC_HEADS, REC_HEAD_DIM)
    lbb = lb_bwd.reshape(REC_HEADS, REC_HEAD_DIM)
    flip = lambda t: jnp.flip(t, axis=1)

    def readout(o, g):
        o = o * lax.rsqrt(jnp.mean(o * o, axis=-1, keepdims=True) + EPS)
        o = o * onorm_g.reshape(REC_HEADS, REC_HEAD_DIM).astype(jnp.float32)
        o = o.reshape(o.shape[0], o.shape[1], d).astype(g.dtype) * jax.nn.silu(g)
        return o @ w_o

    def project_state(p):
        i_, zf, zb = p[..., :d], p[..., d:2 * d], p[..., 2 * d:3 * d]
        kf, lf = _forget(zf, lbf)
        kb, lb = _forget(zb, lbb)
        return _heads(i_), kf, lf, kb, lb

    if ctx_out:
        p_ctx = h_ctx @ w_in
        vc, kfc, lfc, kbc, lbc = project_state(p_ctx)
        qc = jax.nn.silu(_heads(p_ctx[..., 3 * d:4 * d])) * (REC_HEAD_DIM ** -0.5)
        s_zero = jnp.zeros((h_ctx.shape[0], REC_HEADS, REC_HEAD_DIM, REC_HEAD_DIM), jnp.float32)
        oc_f, sc_f = gla_chunked(qc, kfc, vc, lfc, s_zero)
        oc_b, sc_b = gla_chunked(flip(qc), flip(kbc), flip(vc), flip(lbc), s_zero)
        y_ctx = readout(oc_f + flip(oc_b), p_ctx[..., 4 * d:])
    else:
        vc, kfc, lfc, kbc, lbc = project_state(h_ctx @ w_in[:, :3 * d])
        sc_f = gla_final_state(kfc, vc, lfc)
        sc_b = gla_final_state(flip(kbc), flip(vc), flip(lbc))
        y_ctx = None

    p_lat = h_lat @ w_in
    vl, kfl, lfl, kbl, lbl = project_state(p_lat)
    ql = jax.nn.silu(_heads(p_lat[..., 3 * d:4 * d])) * (REC_HEAD_DIM ** -0.5)
    ol_f, _ = gla_chunked(ql, kfl, vl, lfl, sc_f)
    ol_b, _ = gla_chunked(flip(ql), flip(kbl), flip(vl), flip(lbl), sc_b)
    y_lat = readout(ol_f + flip(ol_b), p_lat[..., 4 * d:])
    return y_lat, y_ctx


def setup_inputs(seed: int = 0) -> dict:
    key = jax.random.key(seed)
    ks = jax.random.split(key, 32)
    d, f = D_MODEL, D_FF
    nrm = lambda k, shape, s: jax.random.normal(k, shape, jnp.float32) * s
    return {
        "x": nrm(ks[0], (BATCH, SEQ, d), 1.0),
        "c": nrm(ks[1], (BATCH, d), 1.0),
        "ctx": nrm(ks[2], (BATCH, CTX_LEN, d), 1.0),
        "c_ctx": nrm(ks[3], (d,), 1.0),
        "ada_w": nrm(ks[4], (DEPTH, d, 6 * d), 0.5 * d ** -0.5),
        "ada_b": nrm(ks[5], (DEPTH, 6 * d), 0.02),
        "norm_mix_g": 1.0 + nrm(ks[6], (DEPTH, d), 0.02),
        "norm_ffn_g": 1.0 + nrm(ks[7], (DEPTH, d), 0.02),
        "final_g": 1.0 + nrm(ks[8], (d,), 0.02),
        "conv_pw1_w": nrm(ks[9], (N_CONV_LAYERS, d, 2 * d), d ** -0.5),
        "conv_pw1_b": nrm(ks[10], (N_CONV_LAYERS, 2 * d), 0.02),
        "conv_dw_w": nrm(ks[11], (N_CONV_LAYERS, CONV_WIDTH, d), CONV_WIDTH ** -0.5),
        "conv_dw_b": nrm(ks[12], (N_CONV_LAYERS, d), 0.02),
        "conv_ln_g": 1.0 + nrm(ks[13], (N_CONV_LAYERS, d), 0.02),
        "conv_ln_b": nrm(ks[14], (N_CONV_LAYERS, d), 0.02),
        "conv_pw2_w": nrm(ks[15], (N_CONV_LAYERS, d, d), d ** -0.5),
        "conv_pw2_b": nrm(ks[16], (N_CONV_LAYERS, d), 0.02),
        "rec_w_in": nrm(ks[17], (N_REC_LAYERS, d, 5 * d), d ** -0.5),
        "rec_lb_logits": nrm(ks[18], (2, DEPTH, d), 0.1),
        "rec_onorm_g": 1.0 + nrm(ks[19], (N_REC_LAYERS, d), 0.02),
        "rec_w_o": nrm(ks[20], (N_REC_LAYERS, d, d), d ** -0.5),
        "ffn_w1": nrm(ks[21], (N_CONV_LAYERS, d, f), d ** -0.5),
        "ffn_w3": nrm(ks[22], (N_CONV_LAYERS, d, f), d ** -0.5),
        "ffn_w2": nrm(ks[23], (N_CONV_LAYERS, f, d), f ** -0.5),
        "moe_router": nrm(ks[24], (N_REC_LAYERS, d, N_EXPERTS), d ** -0.5),
        "moe_w1": nrm(ks[25], (N_REC_LAYERS, N_EXPERTS, d, f), d ** -0.5),
        "moe_w3": nrm(ks[26], (N_REC_LAYERS, N_EXPERTS, d, f), d ** -0.5),
        "moe_w2": nrm(ks[27], (N_REC_LAYERS, N_EXPERTS, f, d), f ** -0.5),
    }


def reference(x, c, ctx, c_ctx, ada_w, ada_b, norm_mix_g, norm_ffn_g, final_g,
              conv_pw1_w, conv_pw1_b, conv_dw_w, conv_dw_b, conv_ln_g, conv_ln_b, conv_pw2_w, conv_pw2_b,
              rec_w_in, rec_lb_logits, rec_onorm_g, rec_w_o,
              ffn_w1, ffn_w3, ffn_w2,
              moe_router, moe_w1, moe_w3, moe_w2):
    mod_lat_all = jnp.einsum('bd,lde->lbe', jax.nn.silu(c), ada_w) + ada_b[:, None, :]
    mod_ctx_all = jnp.einsum('d,lde->le', jax.nn.silu(c_ctx), ada_w) + ada_b
    sm = jax.nn.softmax(rec_lb_logits.astype(jnp.float32), axis=1)
    lower = jnp.cumsum(sm, axis=1) - sm[:, :1]

    h_lat, h_ctx = x, ctx
    for i in range(DEPTH):
        last = i == DEPTH - 1
        j = i // N_MIXERS
        ml = jnp.split(mod_lat_all[i][:, None, :], 6, axis=-1)
        mc = jnp.split(mod_ctx_all[i], 6, axis=-1)

        a_lat = modulate(rms_norm(h_lat, norm_mix_g[i]), ml[0], ml[1])
        a_ctx = modulate(rms_norm(h_ctx, norm_mix_g[i]), mc[0], mc[1])
        if i % N_MIXERS == 0:
            cp = (conv_pw1_w[j], conv_pw1_b[j], conv_dw_w[j], conv_dw_b[j],
                  conv_ln_g[j], conv_ln_b[j], conv_pw2_w[j], conv_pw2_b[j])
            y_lat = conv_module(a_lat, axial_dwconv, *cp)
            y_ctx = None if last else conv_module(a_ctx, dwconv1d, *cp)
        else:
            y_lat, y_ctx = hgrn2_mixer(a_lat, a_ctx, rec_w_in[j], lower[0, i], lower[1, i],
                                       rec_onorm_g[j], rec_w_o[j], not last)
        h_lat = h_lat + ml[2] * y_lat
        if not last:
            h_ctx = h_ctx + mc[2] * y_ctx

        f_lat = modulate(rms_norm(h_lat, norm_ffn_g[i]), ml[3], ml[4])
        if last:
            tokens = f_lat
        else:
            f_ctx = modulate(rms_norm(h_ctx, norm_ffn_g[i]), mc[3], mc[4])
            tokens = jnp.concatenate([f_lat, f_ctx], axis=1)
        if i % 2 == 0:
            y = swiglu(tokens, ffn_w1[j], ffn_w3[j], ffn_w2[j])
        else:
            y = moe_swiglu(tokens, moe_router[j], moe_w1[j], moe_w3[j], moe_w2[j])
        s_lat = h_lat.shape[1]
        h_lat = h_lat + ml[5] * y[:, :s_lat]
        if not last:
            h_ctx = h_ctx + mc[5] * y[:, s_lat:]

    return rms_norm(h_lat, final_g)
```

```python
from contextlib import ExitStack, nullcontext
import numpy as np
import concourse.bass as bass
import concourse.mybir as mybir
from concourse.bass_utils import run_bass_kernel_spmd

F32 = mybir.dt.float32
BF16 = mybir.dt.bfloat16
AF = mybir.ActivationFunctionType
ALU = mybir.AluOpType
AX = mybir.AxisListType

NTOK, NLAT, NCTX, D, KC, FF, FC, NE = 2304, 2048, 256, 1024, 8, 2816, 22, 8
DEPTH = 4
EPS = 1e-6
CW = 31
GC = 32
R_C, R_CCTX, R_ADAB, R_NMIX, R_NFFN, R_FING, R_PW1B, R_DWW, R_DWB, R_LNG, R_LNB, R_PW2B, R_LBL, R_ONG = \
    0, 1, 2, 26, 30, 34, 35, 39, 101, 103, 105, 107, 109, 117
C_ID, C_MF, C_MB, C_RST, C_END = 0, 128, 256, 384, 384 + NTOK

NT5 = [(0, 512), (512, 512), (1024, 512), (1536, 512), (2048, 256)]
ST3 = [[(0, 384), (384, 384)], [(768, 384), (1152, 384)], [(1536, 512), (2048, 256)]]


class Buf:
    __slots__ = ("name", "writers", "readers")

    def __init__(self, name):
        self.name = name
        self.writers = {}
        self.readers = {}


class DSem:
    __slots__ = ("key",)

    def __init__(self, key):
        self.key = key


class Ctx:
    ENG = ("pe", "dve", "act", "pool", "sp")
    NPOOL = 90
    NSW = 20

    def __init__(self, nc, stack):
        self.nc = nc
        self.top = stack
        self.stack = stack
        self.eng = {"pe": nc.tensor, "dve": nc.vector, "act": nc.scalar,
                    "pool": nc.gpsimd, "sp": nc.sync}
        self.sems = {}
        self.cnt = {}
        for e in self.ENG:
            self.sems[e] = stack.enter_context(nc.semaphore("c_" + e))
            self.cnt[e] = 0
        self.free_keys = {True: [], False: []}
        for i in range(self.NPOOL):
            k = i
            self.sems[k] = stack.enter_context(nc.semaphore("d%d" % i))
            self.cnt[k] = 0
            self.free_keys[i < self.NSW].append(k)
        self.key_sw = {}
        self.stage_keys = []
        self.waited = {}
        self.uid = 0
        self.n_inst = {e: 0 for e in self.ENG}

    def sbuf(self, name, shape, dt):
        self.uid += 1
        return self.stack.enter_context(self.nc.sbuf_tensor("%s_%d" % (name, self.uid), list(shape), dt))

    def psum(self, name, shape=(128, 512), dt=F32):
        self.uid += 1
        return self.stack.enter_context(self.nc.psum_tensor("%s_%d" % (name, self.uid), list(shape), dt))

    def buf(self, name="b"):
        self.uid += 1
        return Buf("%s%d" % (name, self.uid))

    def bufs(self, n, name="b"):
        return [self.buf(name) for _ in range(n)]

    def dsem(self):
        return DSem(None)

    def _bind(self, sem, q):
        if sem.key is None:
            sw = (q == "pool")
            k = self.free_keys[sw].pop()
            self.key_sw[k] = sw
            self.stage_keys.append(k)
            sem.key = k
        return sem.key

    def _wait(self, e, key, val):
        if val <= 0:
            return
        k = (e, key)
        if self.waited.get(k, 0) >= val:
            return
        if not isinstance(key, str):
            val = max(val, self.cnt[key])
        self.waited[k] = val
        self.eng[e].wait_ge(self.sems[key], val)

    def _need(self, e, reads, writes, same_raw, is_dma=False):
        need = {}
        for b in reads:
            for k, v in b.writers.items():
                if k == e and not same_raw:
                    continue
                need[k] = max(need.get(k, 0), v)
        for b in writes:
            for k, v in b.writers.items():
                if k == e and e == "pe":
                    continue
                need[k] = max(need.get(k, 0), v)
            for k, v in b.readers.items():
                if k == e and e == "pe":
                    continue
                need[k] = max(need.get(k, 0), v)
        for k, v in need.items():
            self._wait(e, k, v)

    def op(self, e, fn, reads=(), writes=(), same_raw=True):
        self._need(e, reads, writes, same_raw)
        inst = fn(self.eng[e])
        self.cnt[e] += 1
        self.n_inst[e] += 1
        inst.then_inc(self.sems[e], 1)
        v = self.cnt[e]
        for b in reads:
            b.readers[e] = v
        for b in writes:
            b.writers = {e: v}
            b.readers = {}
        return inst

    def dma(self, q, sem, pairs, reads, writes, **kw):
        key = self._bind(sem, q)
        self._need(q, reads, writes, True, is_dma=True)
        for (o, i) in pairs:
            self.eng[q].dma_start(out=o, in_=i, **kw).then_inc(self.sems[key], 16)
            self.cnt[key] += 16
            self.n_inst[q] += 1
        v = self.cnt[key]
        for r in reads:
            r.readers[key] = v
        for b in writes:
            b.writers = {key: v}
            b.readers = {}

    def barrier(self):
        keys = list(self.ENG) + list(self.stage_keys)
        for e in self.ENG:
            for k in keys:
                self._wait(e, k, self.cnt[k])

    class _Stage:
        def __init__(self, c):
            self.c = c

        def __enter__(self):
            c = self.c
            self.prev = c.stack
            self.es = ExitStack()
            self.es.__enter__()
            c.stack = self.es
            self.keys0 = len(c.stage_keys)
            return c

        def __exit__(self, *a):
            c = self.c
            c.barrier()
            rel = c.stage_keys[self.keys0:]
            del c.stage_keys[self.keys0:]
            for k in rel:
                c.free_keys[c.key_sw[k]].append(k)
            c.stack = self.prev
            return self.es.__exit__(*a)

    def stage(self):
        return Ctx._Stage(self)


class Rot:
    def __init__(self, c, name, shape, dt, n, kind="sbuf", dma=False):
        self.t = []
        self.b = []
        self.s = []
        for i in range(n):
            self.t.append(c.sbuf(name, shape, dt) if kind == "sbuf" else c.psum(name, shape, dt))
            self.b.append(c.buf(name))
            self.s.append(c.dsem() if dma else None)
        self.i = 0
        self.n = n

    def next(self):
        i = self.i
        self.i = (i + 1) % self.n
        return self.t[i], self.b[i], self.s[i]


def act(c, out, in_, func, reads, writes, **kw):
    return c.op("act", lambda e: e.activation(out=out, in_=in_, func=func, **kw), reads, writes)


def mm(c, out, lhsT, rhs, start, stop, reads, writes):
    return c.op("pe", lambda e: e.matmul(out, lhsT=lhsT, rhs=rhs, start=start, stop=stop), reads, writes)


class Prog:
    def __init__(self, n_stage_list=None, dbg=None):
        self.stages = n_stage_list
        self.dbg = dbg

    def build(self):
        nc = bass.Bass("TRN2", target_bir_lowering=False)
        self.nc = nc
        dt = nc.dram_tensor
        prog = self

        class LazyA(dict):
            def __missing__(self, k):
                v = dt(k, prog.wshape[k], F32, kind="ExternalInput").ap()
                self[k] = v
                return v
        A = LazyA()
        A["xin"] = dt("xin", [NTOK, D], F32, kind="ExternalInput").ap()
        A["vecs"] = dt("vecs", [128, D], F32, kind="ExternalInput").ap()
        A["consts"] = dt("consts", [128, C_END], F32, kind="ExternalInput").ap()
        self.wshape = {"ada_w": [DEPTH, D, 6 * D], "conv_pw1_w": [2, D, 2 * D], "conv_pw2_w": [2, D, D],
                       "rec_w_in": [2, D, 5 * D], "rec_w_o": [2, D, D], "ffn_w1": [2, D, FF], "ffn_w3": [2, D, FF],
                       "ffn_w2": [2, FF, D], "moe_router": [2, D, NE], "moe_w1": [2, NE, D, FF],
                       "moe_w3": [2, NE, D, FF], "moe_w2": [2, NE, FF, D]}
        A["out"] = dt("out", [NLAT, D], F32, kind="ExternalOutput").ap()
        A["Hd"] = dt("Hd", [KC, 128, NTOK], F32, kind="Internal").ap()
        A["Rd"] = dt("Rd", [KC, 128, NTOK], BF16, kind="Internal").ap()
        self.A = A
        with ExitStack() as top:
            c = Ctx(nc, top)
            self.c = c
            self.bHd = c.buf("Hd")
            self.bOut = c.buf("out")
            self.bIn = c.buf("in")
            self.persistent()
            stages = self.stages or (["setup", "input"] +
                                     sum([["mix%d" % i, "ffn%d" % i] for i in range(DEPTH)], []) + ["final"])
            for s in stages:
                if s == "setup":
                    self.stage_setup()
                elif s == "input":
                    if self.stages:
                        self.stage_input()
                elif s.startswith("mix"):
                    i = int(s[3:])
                    if i % 2 == 0:
                        self.stage_conv(i)
                    else:
                        self.stage_hgrn(i)
                elif s.startswith("ffn"):
                    i = int(s[3:])
                    self.stage_ffn(i, moe=(i % 2 == 1))
                elif s == "final":
                    self.stage_final()
                elif s == "dump":
                    self.stage_dump()
                elif s == "dumpctx":
                    self.stage_dump(ctx=True)
            c.barrier()
            for k, v in self.bOut.writers.items():
                c._wait("sp", k, v)
        return nc

    def persistent(self):
        c = self.c
        self.vT = c.sbuf("vT", [128, KC, 128], F32)
        self.modT = c.sbuf("modT", [128, DEPTH, 6, KC, 2], F32)
        self.nmT = c.sbuf("nmT", [128, DEPTH, 2, KC, 2], F32)
        self.lbT = c.sbuf("lbT", [128, KC, 2, 2, 2], F32)
        self.identf = c.sbuf("identf", [128, 128], F32)
        self.identb = c.sbuf("identb", [128, 128], BF16)
        self.onesb = c.sbuf("onesb", [128, 128], BF16)
        self.scb = c.sbuf("scb", [128, KC, 2], BF16)
        self.bP = c.buf("persist")

    def vcol(self, r, k):
        return self.vT[:, k, r:r + 1]

    def stage_setup(self):
        c, A = self.c, self.A
        with c.stage():
            vs = c.sbuf("vs", [128, D], F32)
            bvs = c.buf()
            s0 = c.dsem()
            c.dma("sp", s0, [(vs[:], A["vecs"]), (self.identf[:], A["consts"][:, C_ID:C_ID + 128])],
                  [self.bIn], [bvs])
            c.op("dve", lambda e: e.tensor_copy(out=self.identb[:], in_=self.identf[:]), [bvs], [self.bP])
            c.op("dve", lambda e: e.memset(self.onesb[:], 1.0), [], [self.bP])
            pst = Rot(c, "pst", [128, 512], F32, 2, "psum")
            for k in range(KC):
                ps, bps, _ = pst.next()
                c.op("pe", lambda e: e.transpose(out=ps[:, 0:128], in_=vs[:, k * 128:(k + 1) * 128],
                                                 identity=self.identf[:]), [bvs, self.bP], [bps])
                c.op("dve", lambda e: e.tensor_copy(out=self.vT[:, k, :], in_=ps[:, 0:128]), [bps], [self.bP])
            act(c, self.scb[:], self.vT[:, :, R_C:R_C + 2], AF.Silu, [self.bP], [self.bP])
            if not self.stages:
                self.stage_input(own_stage=False)
            mps = Rot(c, "mps", [128, 512], F32, 2, "psum")
            for li in (range(DEPTH) if self.stages else range(1)):
                for th in self.adaln_pieces(li, mps):
                    th()
            ex = c.sbuf("ex", [128, KC, 8], F32)
            bex = c.buf()
            act(c, ex[:], self.vT[:, :, R_LBL:R_LBL + 8], AF.Exp, [self.bP], [bex])
            sm = c.sbuf("sm", [128, KC, 2], F32)
            for d in range(2):
                c.op("dve", lambda e: e.tensor_reduce(out=sm[:, :, d], in_=ex[:, :, d * 4:(d + 1) * 4],
                                                      axis=AX.X, op=ALU.add), [bex], [bex])
            c.op("dve", lambda e: e.reciprocal(out=sm[:], in_=sm[:]), [bex], [bex])
            for d in range(2):
                c.op("dve", lambda e: e.tensor_tensor(out=self.lbT[:, :, d, 0, 0], in0=ex[:, :, d * 4 + 1],
                                                      in1=sm[:, :, d], op=ALU.mult), [bex], [self.bP])
                c.op("dve", lambda e: e.tensor_tensor(out=self.lbT[:, :, d, 1, 1], in0=ex[:, :, d * 4 + 0],
                                                      in1=sm[:, :, d], op=ALU.mult), [bex], [self.bP])
                c.op("dve", lambda e: e.tensor_scalar(out=self.lbT[:, :, d, 0, 1], in0=self.lbT[:, :, d, 0, 0],
                                                      scalar1=-1.0, scalar2=1.0, op0=ALU.mult, op1=ALU.add),
                     [self.bP], [self.bP])
                c.op("dve", lambda e: e.tensor_scalar(out=self.lbT[:, :, d, 1, 0], in0=self.lbT[:, :, d, 1, 1],
                                                      scalar1=-1.0, scalar2=1.0, op0=ALU.mult, op1=ALU.add),
                     [self.bP], [self.bP])

    def adaln_pieces(self, i, mps):
        c, A = self.c, self.A
        wrot = Rot(c, "adaw", [128, 3, 1024], BF16, 2, dma=True)
        acc = c.sbuf("acc", [128, 48, 2], F32)
        bacc = c.buf()
        out = []

        def piece(k, half):
            def run():
                wt, bw, sw = wrot.next()
                src = A["ada_w"][i, k * 128:(k + 1) * 128, half * 3072:(half + 1) * 3072]
                c.dma("pool", sw, [(wt[:], src.rearrange("p (a b) -> p a b", b=1024))], [self.bIn], [bw])
                ps, bps, _ = mps.next()
                psv = ps[:, 0:48].rearrange("p (a b) -> p a b", b=2)
                for ec in range(24):
                    a_, b_ = divmod(ec * 128, 1024)
                    mm(c, psv[:, ec, :], wt[:, a_, b_:b_ + 128], self.scb[:, k, :], True, True,
                       [bw, self.bP], [bps])
                dst = acc[:, half * 24:(half + 1) * 24, :]
                if k == 0:
                    c.op("dve", lambda e: e.tensor_copy(out=dst, in_=psv[:, 0:24, :]), [bps], [bacc])
                else:
                    c.op("dve", lambda e: e.tensor_tensor(out=dst, in0=dst, in1=psv[:, 0:24, :], op=ALU.add),
                         [bps, bacc], [bacc])
            return run

        for k in range(KC):
            for half in range(2):
                out.append(piece(k, half))

        def fin():
            for part in range(6):
                for x in range(2):
                    c.op("dve", lambda e: e.tensor_tensor(
                        out=self.modT[:, i, part, :, x], in0=acc[:, part * 8:(part + 1) * 8, x],
                        in1=self.vT[:, :, R_ADAB + i * 6 + part], op=ALU.add), [bacc, self.bP], [self.bP])
            for which, (rg, part) in enumerate(((R_NMIX + i, 1), (R_NFFN + i, 4))):
                for x in range(2):
                    c.op("dve", lambda e: e.scalar_tensor_tensor(
                        out=self.nmT[:, i, which, :, x], in0=self.modT[:, i, part, :, x], scalar=1.0,
                        in1=self.vT[:, :, rg], op0=ALU.add, op1=ALU.mult), [self.bP], [self.bP])
        out.append(fin)
        return out

    def stage_input(self, own_stage=True):
        c, A = self.c, self.A
        with (c.stage() if own_stage else nullcontext()):
            xr = Rot(c, "xin", [128, D], F32, 3, dma=True)
            pr = Rot(c, "pin", [128, 512], F32, 4, "psum")
            hr = Rot(c, "hin", [128, KC, 128], F32, 3, dma=True)
            for tb in range(NTOK // 128):
                xt, bx, sx = xr.next()
                c.dma("sp", sx, [(xt[:], A["xin"][tb * 128:(tb + 1) * 128, :])], [self.bIn], [bx])
                ht, bh, sh = hr.next()
                for g in range(2):
                    ps, bps, _ = pr.next()
                    for q in range(4):
                        k = g * 4 + q
                        c.op("pe", lambda e: e.transpose(out=ps[:, q * 128:(q + 1) * 128],
                                                         in_=xt[:, k * 128:(k + 1) * 128],
                                                         identity=self.identf[:]), [bx, self.bP], [bps])
                    eng = "dve" if g == 0 else "act"
                    dst = ht[:, g * 4:(g + 1) * 4, :]
                    src = ps[:].rearrange("p (a b) -> p a b", b=128)
                    if eng == "dve":
                        c.op("dve", lambda e: e.tensor_copy(out=dst, in_=src), [bps], [bh])
                    else:
                        c.op("act", lambda e: e.copy(out=dst, in_=src), [bps], [bh])
                c.dma("sp", sh, [(A["Hd"][:, :, tb * 128:(tb + 1) * 128].rearrange("k p t -> p k t"), ht[:])],
                      [bh], [c.buf()])

    def norm_tile(self, ht, bh, w, nm, sh, out, bout, tmp_rot, sq_rot, ps_rot, rs_rot, out32=None):
        c = self.c
        sq, bsq, _ = sq_rot.next()
        for k in range(KC):
            act(c, sq[:, k, 0:w], ht(k), AF.Square, bh, [bsq])
        ps, bps, _ = ps_rot.next()
        for k in range(KC):
            mm(c, ps[:, 0:w], self.onesb[:], sq[:, k, 0:w], k == 0, k == KC - 1, [bsq, self.bP], [bps])
        rs, brs, _ = rs_rot.next()
        act(c, rs[:, 0:w], ps[:, 0:w], AF.Ln, [bps], [brs], scale=1.0 / D, bias=self.epsc[:, 0:1])
        act(c, rs[:, 0:w], rs[:, 0:w], AF.Exp, [brs], [brs], scale=-0.5)
        for k in range(KC):
            tmp, btmp, _ = tmp_rot.next()
            c.op("dve", lambda e: e.tensor_tensor(out=tmp[:, 0:w], in0=ht(k), in1=rs[:, 0:w], op=ALU.mult),
                 list(bh) + [brs], [btmp])
            if sh is not None:
                c.op("dve", lambda e: e.tensor_scalar(out=out(k), in0=tmp[:, 0:w], scalar1=nm(k), scalar2=sh(k),
                                                      op0=ALU.mult, op1=ALU.add), [btmp, self.bP], bout)
            else:
                c.op("dve", lambda e: e.tensor_scalar(out=out(k), in0=tmp[:, 0:w], scalar1=nm(k), scalar2=None,
                                                      op0=ALU.mult), [btmp, self.bP], bout)
            if out32 is not None:
                c.op("pool", lambda e: e.tensor_scalar(out=out32(k), in0=tmp[:, 0:w], scalar1=nm(k), scalar2=sh(k),
                                                       op0=ALU.mult, op1=ALU.add), [btmp, self.bP], bout)

    def eps_const(self):
        c = self.c
        self.epsc = c.sbuf("epsc", [128, 1], F32)
        c.op("dve", lambda e: e.memset(self.epsc[:], EPS), [], [self.bP])

    def stage_ffn(self, i, moe):
        c, A = self.c, self.A
        j = i // 2
        last = (i == DEPTH - 1)
        with c.stage():
            self.eps_const()
            Hst = c.sbuf("Hst", [128, KC, 768], F32)
            F = c.sbuf("F", [128, KC, 768], BF16)
            G = c.sbuf("G", [128, FC, 768], BF16)
            sH = c.dsem()
            sSt = c.dsem()
            tmp_rot = Rot(c, "ntmp", [128, 512], F32, 2)
            sq_rot = Rot(c, "nsq", [128, KC, 512], BF16, 1)
            rs_rot = Rot(c, "nrs", [128, 512], F32, 1)
            ps_n = Rot(c, "psn", [128, 512], F32, 1, "psum")
            ps1 = Rot(c, "ps1", [128, 512], F32, 2, "psum")
            ps3 = Rot(c, "ps3", [128, 512], F32, 2, "psum")
            ps2 = Rot(c, "ps2", [128, 512], F32, 2, "psum")
            w1r = Rot(c, "w1", [128, KC, 512], BF16, 2, dma=True)
            w3r = Rot(c, "w3", [128, KC, 512], BF16, 2, dma=True)
            w2r = Rot(c, "w2", [128, FC, 256], BF16, 2, dma=True)
            sr = Rot(c, "sil", [128, 512], F32, 3)
            if moe:
                F32t = c.sbuf("F32t", [128, KC, 768], F32)
                gateB = c.sbuf("gateB", [128, NE, 768], BF16)
                rt = c.sbuf("rt", [128, KC, NE], F32)
                brt = c.buf()
                srt = c.dsem()
                c.dma("sp", srt, [(rt[:], A["moe_router"][j].rearrange("(k p) e -> p k e", p=128))],
                      [self.bIn], [brt])
                psr = Rot(c, "psr", [128, 512], F32, 1, "psum")
                Lg = c.sbuf("Lg", [128, 6, 8], F32)
                S8 = c.sbuf("S8", [128, 6, 8], F32)
                Gt = c.sbuf("Gt", [128, 6, 8], F32)
                Et = c.sbuf("Et", [128, 6, 8], F32)
                Wg = c.sbuf("Wg", [128, 6, 2], F32)
                bLg, bS8, bGt, bEt, bWg = c.buf(), c.buf(), c.buf(), c.buf(), c.buf()
                dgm = Rot(c, "dgm", [128, 128], BF16, 2)
                sgr = Rot(c, "sgr", [128, 512], BF16, 3)
            items = []
            for st, tiles in enumerate(ST3):
                if last:
                    tiles = [t for t in tiles if t[0] < NLAT]
                items.append(("pre", st, tiles, None, None))
                for ex in range(NE if moe else 1):
                    for jb in range(6):
                        items.append(("p1", st, tiles, ex, jb))
                    for mp in range(4):
                        items.append(("p2", st, tiles, ex, mp))
                items.append(("post", st, tiles, None, None))
            loaded = {}
            S = {}
            PB = dict(bHt=[[c.buf() for _ in range(2)] for _ in range(KC)], bF=[c.buf() for _ in range(2)],
                      bG=[[c.buf() for _ in range(2)] for _ in range(FC)], bGB=[c.buf() for _ in range(2)])

            def weights(ex):
                if moe:
                    return A["moe_w1"][j, ex], A["moe_w3"][j, ex], A["moe_w2"][j, ex]
                return A["ffn_w1"][j], A["ffn_w3"][j], A["ffn_w2"][j]

            def load(idx):
                kind, st, tiles, ex, q = items[idx]
                if idx in loaded or kind in ("pre", "post"):
                    return
                W1, W3, W2 = weights(ex)
                if kind == "p1":
                    nf = 4 if q < 5 else 2
                    w1t, bw1, sw1 = w1r.next()
                    w3t, bw3, sw3 = w3r.next()
                    cs = slice(q * 512, q * 512 + nf * 128)
                    c.dma("pool", sw1, [(w1t[:, :, 0:nf * 128], W1[:, cs].rearrange("(k p) f -> p k f", p=128))],
                          [self.bIn], [bw1])
                    c.dma("pool", sw3, [(w3t[:, :, 0:nf * 128], W3[:, cs].rearrange("(k p) f -> p k f", p=128))],
                          [self.bIn], [bw3])
                    loaded[idx] = (w1t, bw1, w3t, bw3)
                else:
                    w2t, bw2, sw2 = w2r.next()
                    c.dma("pool", sw2, [(w2t[:], W2[:, q * 256:(q + 1) * 256].rearrange("(f p) c -> p f c", p=128))],
                          [self.bIn], [bw2])
                    loaded[idx] = (w2t, bw2)

            def pre(st, tiles):
                n0s = tiles[0][0]
                wst = sum(t[1] for t in tiles)
                bHt, bF, bG, bGB = PB["bHt"], PB["bF"], PB["bG"], PB["bGB"]
                allH = [b for row in bHt for b in row]
                S.update(n0s=n0s, wst=wst, bHt=bHt, allH=allH, bF=bF, bG=bG, bGB=bGB)
                fence = allH + bF + [b for row in bG for b in row] + bGB
                c.dma("sp", sH, [(Hst[:, :, 0:wst], A["Hd"][:, :, n0s:n0s + wst].rearrange("k p t -> p k t"))],
                      [self.bIn], fence)
                for nt, (n0, w) in enumerate(tiles):
                    o = n0 - n0s
                    x = 0 if n0 < NLAT else 1
                    self.norm_tile(lambda k: Hst[:, k, o:o + w], [bHt[k][nt] for k in range(KC)], w,
                                   lambda k: self.nmT[:, i, 1, k, x:x + 1], lambda k: self.modT[:, i, 3, k, x:x + 1],
                                   lambda k: F[:, k, o:o + w], [bF[nt]], tmp_rot, sq_rot, ps_n, rs_rot,
                                   out32=(lambda k: F32t[:, k, o:o + w]) if moe else None)
                if moe:
                    blocks = [(nt, n0 - n0s + tb * 128) for nt, (n0, w) in enumerate(tiles) for tb in range(w // 128)]
                    nb6 = len(blocks)
                    ps, bps, _ = psr.next()
                    for bi, (nt, off) in enumerate(blocks):
                        for k in range(KC):
                            mm(c, ps[:, bi * 8:(bi + 1) * 8], F32t[:, k, off:off + 128], rt[:, k, :],
                               k == 0, k == KC - 1, [bF[nt], brt], [bps])
                    L3 = Lg[:, 0:nb6, :]
                    c.op("dve", lambda e: e.tensor_copy(out=L3, in_=ps[:, 0:nb6 * 8].rearrange("p (a b) -> p a b", b=8)),
                         [bps], [bLg])
                    for bi in range(nb6):
                        c.op("dve", lambda e: e.max(out=S8[:, bi, :], in_=Lg[:, bi, :]), [bLg], [bS8])
                    c.op("dve", lambda e: e.tensor_tensor(out=Wg[:, 0:nb6, 0], in0=S8[:, 0:nb6, 0], in1=S8[:, 0:nb6, 1],
                                                          op=ALU.subtract), [bS8], [bWg])
                    act(c, Wg[:, 0:nb6, 0], Wg[:, 0:nb6, 0], AF.Sigmoid, [bWg], [bWg])
                    c.op("dve", lambda e: e.tensor_scalar(out=Wg[:, 0:nb6, 1], in0=Wg[:, 0:nb6, 0], scalar1=-1.0,
                                                          scalar2=1.0, op0=ALU.mult, op1=ALU.add), [bWg], [bWg])
                    shp = [128, nb6, 8]
                    G3, E3 = Gt[:, 0:nb6, :], Et[:, 0:nb6, :]
                    c.op("dve", lambda e: e.tensor_tensor(out=G3, in0=L3, in1=S8[:, 0:nb6, 0:1].broadcast_to(shp),
                                                          op=ALU.is_equal), [bLg, bS8], [bGt])
                    c.op("dve", lambda e: e.tensor_tensor(out=G3, in0=G3, in1=Wg[:, 0:nb6, 0:1].broadcast_to(shp),
                                                          op=ALU.mult), [bGt, bWg], [bGt])
                    c.op("dve", lambda e: e.tensor_tensor(out=E3, in0=L3, in1=S8[:, 0:nb6, 1:2].broadcast_to(shp),
                                                          op=ALU.is_equal), [bLg, bS8], [bEt])
                    c.op("dve", lambda e: e.tensor_tensor(out=E3, in0=E3, in1=Wg[:, 0:nb6, 1:2].broadcast_to(shp),
                                                          op=ALU.mult), [bEt, bWg], [bEt])
                    c.op("dve", lambda e: e.tensor_tensor(out=G3, in0=G3, in1=E3, op=ALU.add), [bGt, bEt], [bGt])
                    for bi, (nt, off) in enumerate(blocks):
                        for ex in range(NE):
                            dm, bdm, _ = dgm.next()
                            c.op("dve", lambda e: e.tensor_scalar(out=dm[:], in0=self.identb[:],
                                                                  scalar1=Gt[:, bi, ex:ex + 1], scalar2=None,
                                                                  op0=ALU.mult), [bGt, self.bP], [bdm])
                            ps_, bps_, _ = psr.next()
                            mm(c, ps_[:, 0:128], self.onesb[:], dm[:], True, True, [bdm, self.bP], [bps_])
                            c.op("act", lambda e: e.copy(out=gateB[:, ex, off:off + 128], in_=ps_[:, 0:128]),
                                 [bps_], [bGB[nt]])

            def phase1(idx, tiles, ex, jb):
                w1t, bw1, w3t, bw3 = loaded.pop(idx)
                n0s, bF, bG, bGB = S["n0s"], S["bF"], S["bG"], S["bGB"]
                nf = 4 if jb < 5 else 2
                for q in range(nf):
                    f = jb * 4 + q
                    for nt, (n0, w) in enumerate(tiles):
                        o = n0 - n0s
                        p1, bp1, _ = ps1.next()
                        p3, bp3, _ = ps3.next()
                        for k in range(KC):
                            mm(c, p1[:, 0:w], w1t[:, k, q * 128:(q + 1) * 128], F[:, k, o:o + w],
                               k == 0, k == KC - 1, [bw1, bF[nt]], [bp1])
                        for k in range(KC):
                            mm(c, p3[:, 0:w], w3t[:, k, q * 128:(q + 1) * 128], F[:, k, o:o + w],
                               k == 0, k == KC - 1, [bw3, bF[nt]], [bp3])
                        s_, bs_, _ = sr.next()
                        act(c, s_[:, 0:w], p1[:, 0:w], AF.Silu, [bp1], [bs_])
                        if moe:
                            sg_, bsg_, _ = sgr.next()
                            c.op("dve", lambda e: e.tensor_tensor(out=sg_[:, 0:w], in0=s_[:, 0:w],
                                                                  in1=p3[:, 0:w], op=ALU.mult),
                                 [bs_, bp3], [bsg_])
                            c.op("pool", lambda e: e.tensor_tensor(out=G[:, f, o:o + w], in0=sg_[:, 0:w],
                                                                   in1=gateB[:, ex, o:o + w], op=ALU.mult),
                                 [bsg_, bGB[nt]], [bG[f][nt]])
                        else:
                            c.op("dve", lambda e: e.tensor_tensor(out=G[:, f, o:o + w], in0=s_[:, 0:w],
                                                                  in1=p3[:, 0:w], op=ALU.mult),
                                 [bs_, bp3], [bG[f][nt]])

            def phase2(idx, tiles, ex, mp):
                w2t, bw2 = loaded.pop(idx)
                n0s, bG, bHt = S["n0s"], S["bG"], S["bHt"]
                for mm_ in range(2):
                    m = mp * 2 + mm_
                    for nt, (n0, w) in enumerate(tiles):
                        o = n0 - n0s
                        x = 0 if n0 < NLAT else 1
                        p2, bp2, _ = ps2.next()
                        for f in range(FC):
                            mm(c, p2[:, 0:w], w2t[:, f, mm_ * 128:(mm_ + 1) * 128], G[:, f, o:o + w],
                               f == 0, f == FC - 1, [bw2, bG[f][nt]], [bp2])
                        c.op("dve", lambda e: e.scalar_tensor_tensor(
                            out=Hst[:, m, o:o + w], in0=p2[:, 0:w], scalar=self.modT[:, i, 5, m, x:x + 1],
                            in1=Hst[:, m, o:o + w], op0=ALU.mult, op1=ALU.add),
                            [bp2, bHt[m][nt], self.bP], [bHt[m][nt]])

            def post():
                n0s, wst = S["n0s"], S["wst"]
                c.dma("sp", sSt, [(A["Hd"][:, :, n0s:n0s + wst].rearrange("k p t -> p k t"), Hst[:, :, 0:wst])],
                      S["allH"], [c.buf()])

            widx = [q for q, it in enumerate(items) if it[0] in ("p1", "p2")]
            ada = self.adaln_pieces(i + 1, ps_n) if (i + 1 < DEPTH and not self.stages) else []
            every = max(1, (len(items) - 4) // (len(ada) + 1)) if ada else 0
            for idx, (kind, st, tiles, ex, q) in enumerate(items):
                if ada and kind in ("p1", "p2") and idx % every == 0:
                    ada.pop(0)()
                nxt = [w_ for w_ in widx if w_ >= idx][:2]
                if kind in ("p1", "p2"):
                    for w_ in nxt:
                        load(w_)
                else:
                    for w_ in nxt[:1]:
                        load(w_)
                if kind == "pre":
                    pre(st, tiles)
                elif kind == "p1":
                    phase1(idx, tiles, ex, q)
                elif kind == "p2":
                    phase2(idx, tiles, ex, q)
                else:
                    post()
            while ada:
                ada.pop(0)()

    def stage_final(self):
        c, A = self.c, self.A
        with c.stage():
            self.eps_const()
            hr = Rot(c, "fh", [128, KC, 512], F32, 2, dma=True)
            o32 = Rot(c, "fo", [128, KC, 512], F32, 2)
            tmp_rot = Rot(c, "ntmp", [128, 512], F32, 2)
            sq_rot = Rot(c, "nsq", [128, KC, 512], BF16, 1)
            rs_rot = Rot(c, "nrs", [128, 512], F32, 1)
            ps_n = Rot(c, "psn", [128, 512], F32, 1, "psum")
            pr = Rot(c, "pout", [128, 512], F32, 4, "psum")
            orow = Rot(c, "orow", [128, D], F32, 3, dma=True)
            for nt, (n0, w) in enumerate(NT5[:4]):
                ht, bh, sh = hr.next()
                c.dma("sp", sh, [(ht[:, :, 0:w], A["Hd"][:, :, n0:n0 + w].rearrange("k p t -> p k t"))],
                      [self.bIn], [bh])
                ot, bo, _ = o32.next()
                self.norm_tile(lambda k: ht[:, k, 0:w], [bh], w, lambda k: self.vcol(R_FING, k), None,
                               lambda k: ot[:, k, 0:w], [bo], tmp_rot, sq_rot, ps_n, rs_rot)
                for tb in range(w // 128):
                    rw, brw, srw = orow.next()
                    for g in range(2):
                        ps, bps, _ = pr.next()
                        for q in range(4):
                            k = g * 4 + q
                            c.op("pe", lambda e: e.transpose(out=ps[:, q * 128:(q + 1) * 128],
                                                             in_=ot[:, k, tb * 128:(tb + 1) * 128],
                                                             identity=self.identf[:]), [bo, self.bP], [bps])
                        if g == 0:
                            c.op("dve", lambda e: e.tensor_copy(out=rw[:, 0:512], in_=ps[:]), [bps], [brw])
                        else:
                            c.op("act", lambda e: e.copy(out=rw[:, 512:1024], in_=ps[:]), [bps], [brw])
                    c.dma("sp", srw, [(A["out"][n0 + tb * 128:n0 + (tb + 1) * 128, :], rw[:])], [brw], [self.bOut])

    def stage_dump(self, ctx=False):
        c, A = self.c, self.A
        tl = [(2048, 256, 0)] if ctx else [(n0, w, n0) for (n0, w) in NT5[:4]]
        with c.stage():
            hr = Rot(c, "fh", [128, KC, 512], F32, 2, dma=True)
            pr = Rot(c, "pout", [128, 512], F32, 4, "psum")
            orow = Rot(c, "orow", [128, D], F32, 3, dma=True)
            for nt, (n0, w, o0) in enumerate(tl):
                ht, bh, sh = hr.next()
                c.dma("sp", sh, [(ht[:, :, 0:w], A["Hd"][:, :, n0:n0 + w].rearrange("k p t -> p k t"))],
                      [self.bIn], [bh])
                for tb in range(w // 128):
                    rw, brw, srw = orow.next()
                    for g in range(2):
                        ps, bps, _ = pr.next()
                        for q in range(4):
                            k = g * 4 + q
                            c.op("pe", lambda e: e.transpose(out=ps[:, q * 128:(q + 1) * 128],
                                                             in_=ht[:, k, tb * 128:(tb + 1) * 128],
                                                             identity=self.identf[:]), [bh, self.bP], [bps])
                        if g == 0:
                            c.op("dve", lambda e: e.tensor_copy(out=rw[:, 0:512], in_=ps[:]), [bps], [brw])
                        else:
                            c.op("act", lambda e: e.copy(out=rw[:, 512:1024], in_=ps[:]), [bps], [brw])
                    c.dma("sp", srw, [(A["out"][o0 + tb * 128:o0 + (tb + 1) * 128, :], rw[:])], [brw], [self.bOut])

    def stage_conv(self, i):
        c, A = self.c, self.A
        j = i // 2
        with c.stage():
            self.eps_const()
            Aact = c.sbuf("A", [128, KC, NTOK], BF16)
            V = c.sbuf("V", [128, KC, NTOK], BF16)
            bA = [c.buf() for _ in NT5]
            bV = [[c.buf() for _ in NT5] for _ in range(KC)]
            bS = [c.buf() for _ in NT5]
            with c.stage():
                hr = Rot(c, "ch", [128, KC, 512], F32, 2, dma=True)
                tmp_rot = Rot(c, "ntmp", [128, 512], F32, 2)
                sq_rot = Rot(c, "nsq", [128, KC, 512], BF16, 1)
                rs_rot = Rot(c, "nrs", [128, 512], F32, 1)
                ps_n = Rot(c, "psn", [128, 512], F32, 2, "psum")
                for nt, (n0, w) in enumerate(NT5):
                    x = 0 if n0 < NLAT else 1
                    ht, bh, sh = hr.next()
                    c.dma("sp", sh, [(ht[:, :, 0:w], A["Hd"][:, :, n0:n0 + w].rearrange("k p t -> p k t"))],
                          [self.bIn], [bh])
                    self.norm_tile(lambda k: ht[:, k, 0:w], [bh], w,
                                   lambda k: self.nmT[:, i, 0, k, x:x + 1], lambda k: self.modT[:, i, 0, k, x:x + 1],
                                   lambda k: Aact[:, k, n0:n0 + w], [bA[nt]], tmp_rot, sq_rot, ps_n, rs_rot)
            with c.stage():
                LW, LH = 32 * 94, 62 * 64
                upW = Rot(c, "upW", [128, LW + 286], BF16, 2)
                upH = Rot(c, "upH", [128, LH + 286], BF16, 2)
                for r_ in (upW, upH):
                    for q in range(2):
                        c.op("pool", lambda e: e.memset(r_.t[q][:], 0.0), [], [r_.b[q]])
                dgr = Rot(c, "dg", [128, CW, 128], BF16, 2)
                w1r = Rot(c, "cw1", [128, 2, KC, 128], BF16, 2, dma=True)
                sgr = Rot(c, "sg", [128, 512], F32, 2)
                psl = Rot(c, "psl", [128, 512], F32, 2, "psum")
                psg = Rot(c, "psg", [128, 512], F32, 2, "psum")
                psc = Rot(c, "psc", [128, 512], F32, 3, "psum")
                W1 = A["conv_pw1_w"][j]
                for ch in range(KC):
                    wtype = ch < 4
                    up, bup, _ = (upW if wtype else upH).next()
                    LB = LW if wtype else LH
                    w1t, bw1, sw1 = w1r.next()
                    c.dma("pool", sw1,
                          [(w1t[:, 0], W1[:, ch * 128:(ch + 1) * 128].rearrange("(k p) c -> p k c", p=128)),
                           (w1t[:, 1], W1[:, D + ch * 128:D + (ch + 1) * 128].rearrange("(k p) c -> p k c", p=128))],
                          [self.bIn], [bw1])
                    dg, bdg, _ = dgr.next()
                    for k in range(CW):
                        c.op("dve", lambda e: e.tensor_scalar(out=dg[:, k, :], in0=self.identb[:],
                                                              scalar1=self.vcol(R_DWW + j * CW + k, ch), scalar2=None,
                                                              op0=ALU.mult), [self.bP], [bdg])

                    def upview(n0, w, k):
                        if n0 >= NLAT:
                            return up[:, LB + k:LB + k + 256], False
                        r0 = n0 // 64
                        if wtype:
                            v = up[:, r0 * 94:(r0 + 8) * 94].rearrange("p (r q) -> p r q", q=94)
                            return v[:, :, k:k + 64], True
                        return up[:, (r0 + k) * 64:(r0 + k) * 64 + 512], False

                    for nt, (n0, w) in enumerate(NT5):
                        pl, bpl, _ = psl.next()
                        pg, bpg, _ = psg.next()
                        for k in range(KC):
                            mm(c, pl[:, 0:w], w1t[:, 0, k, :], Aact[:, k, n0:n0 + w], k == 0, k == KC - 1,
                               [bw1, bA[nt]], [bpl])
                        for k in range(KC):
                            mm(c, pg[:, 0:w], w1t[:, 1, k, :], Aact[:, k, n0:n0 + w], k == 0, k == KC - 1,
                               [bw1, bA[nt]], [bpg])
                        sg, bsg, _ = sgr.next()
                        act(c, sg[:, 0:w], pg[:, 0:w], AF.Sigmoid, [bpg, self.bP], [bsg],
                            bias=self.vcol(R_PW1B + j * 2 + 1, ch))
                        ov, is3 = upview(n0, w, 15)
                        if is3:
                            i0 = pl[:, 0:w].rearrange("p (r q) -> p r q", q=64)
                            i1 = sg[:, 0:w].rearrange("p (r q) -> p r q", q=64)
                        else:
                            i0, i1 = pl[:, 0:w], sg[:, 0:w]
                        c.op("dve", lambda e: e.scalar_tensor_tensor(
                            out=ov, in0=i0, scalar=self.vcol(R_PW1B + j * 2, ch), in1=i1,
                            op0=ALU.add, op1=ALU.mult), [bpl, bsg, self.bP], [bup])
                    for nt, (n0, w) in enumerate(NT5):
                        pc, bpc, _ = psc.next()
                        for k in range(CW):
                            rv, is3 = upview(n0, w, k)
                            ov = pc[:, 0:w].rearrange("p (r q) -> p r q", q=64) if is3 else pc[:, 0:w]
                            mm(c, ov, dg[:, k, :], rv, k == 0, k == CW - 1, [bdg, bup], [bpc])
                        act(c, V[:, ch, n0:n0 + w], pc[:, 0:w], AF.Identity, [bpc, self.bP], [bV[ch][nt]],
                            bias=self.vcol(R_DWB + j, ch))
            with c.stage():
                sq_rot = Rot(c, "lsq", [128, KC, 512], BF16, 2)
                ps_s = Rot(c, "pss", [128, 512], F32, 2, "psum")
                ps_q = Rot(c, "psq", [128, 512], F32, 2, "psum")
                mr = Rot(c, "lmean", [128, 512], F32, 2)
                vr = Rot(c, "lvar", [128, 512], F32, 2)
                t1r = Rot(c, "lt1", [128, 512], F32, 3)
                for nt, (n0, w) in enumerate(NT5):
                    sq, bsq, _ = sq_rot.next()
                    allV = [bV[k][nt] for k in range(KC)]
                    for k in range(KC):
                        act(c, sq[:, k, 0:w], V[:, k, n0:n0 + w], AF.Square, [bV[k][nt]], [bsq])
                    pS, bpS, _ = ps_s.next()
                    pQ, bpQ, _ = ps_q.next()
                    for k in range(KC):
                        mm(c, pS[:, 0:w], self.onesb[:], V[:, k, n0:n0 + w], k == 0, k == KC - 1,
                           [bV[k][nt], self.bP], [bpS])
                    for k in range(KC):
                        mm(c, pQ[:, 0:w], self.onesb[:], sq[:, k, 0:w], k == 0, k == KC - 1, [bsq, self.bP], [bpQ])
                    mean, bm, _ = mr.next()
                    var, bv, _ = vr.next()
                    act(c, mean[:, 0:w], pS[:, 0:w], AF.Identity, [bpS], [bm], scale=1.0 / D)
                    c.op("dve", lambda e: e.tensor_tensor(out=var[:, 0:w], in0=mean[:, 0:w], in1=mean[:, 0:w],
                                                          op=ALU.mult), [bm], [bv])
                    c.op("dve", lambda e: e.scalar_tensor_tensor(out=var[:, 0:w], in0=pQ[:, 0:w], scalar=1.0 / D,
                                                                 in1=var[:, 0:w], op0=ALU.mult, op1=ALU.subtract),
                         [bpQ, bv], [bv])
                    act(c, var[:, 0:w], var[:, 0:w], AF.Ln, [bv], [bv], bias=self.epsc[:, 0:1])
                    act(c, var[:, 0:w], var[:, 0:w], AF.Exp, [bv], [bv], scale=-0.5)
                    for k in range(KC):
                        t1, bt1, _ = t1r.next()
                        c.op("dve", lambda e: e.tensor_tensor(out=t1[:, 0:w], in0=V[:, k, n0:n0 + w],
                                                              in1=mean[:, 0:w], op=ALU.subtract),
                             [bV[k][nt], bm], [bt1])
                        c.op("dve", lambda e: e.tensor_tensor(out=t1[:, 0:w], in0=t1[:, 0:w], in1=var[:, 0:w],
                                                              op=ALU.mult), [bt1, bv], [bt1])
                        act(c, Aact[:, k, n0:n0 + w], t1[:, 0:w], AF.Silu, [bt1, self.bP], [bS[nt]],
                            scale=self.vcol(R_LNG + j, k), bias=self.vcol(R_LNB + j, k))
            with c.stage():
                w2 = c.sbuf("cw2", [128, KC, D], BF16)
                bw2 = c.buf()
                sw2 = c.dsem()
                c.dma("pool", sw2, [(w2[:], A["conv_pw2_w"][j].rearrange("(k p) m -> p k m", p=128))],
                      [self.bIn], [bw2])
                self.residual_out(i, 2, w2, bw2, Aact, bS, NT5, bias_row=R_PW2B + j)

    def residual_out(self, i, gpart, w2, bw2, S, bS, tiles, bias_row=None):
        c, A = self.c, self.A
        bgc = c.sbuf("bgc", [128, KC, 2], F32)
        bbg = c.buf()
        if bias_row is not None:
            for x in range(2):
                c.op("dve", lambda e: e.tensor_tensor(out=bgc[:, :, x], in0=self.vT[:, :, bias_row],
                                                      in1=self.modT[:, i, gpart, :, x], op=ALU.mult),
                     [self.bP], [bbg])
        hr = Rot(c, "rh", [128, 512], F32, 4, dma=True)
        hs = [c.dsem() for _ in range(4)]
        tr = Rot(c, "rtmp", [128, 512], F32, 2)
        ps = Rot(c, "rps", [128, 512], F32, 3, "psum")
        items = [(nt, n0, w, m) for nt, (n0, w) in enumerate(tiles) for m in range(KC)]
        slots = {}

        def load(idx):
            nt, n0, w, m = items[idx]
            ht, bh, sh = hr.next()
            slots[idx] = (ht, bh, hs[idx % 4])
            c.dma("sp", sh, [(ht[:, 0:w], A["Hd"][m, :, n0:n0 + w])], [self.bIn], [bh])

        for idx in range(min(2, len(items))):
            load(idx)
        for idx, (nt, n0, w, m) in enumerate(items):
            if idx + 2 < len(items):
                load(idx + 2)
            x = 0 if n0 < NLAT else 1
            ht, bh, sst = slots.pop(idx)
            p, bp, _ = ps.next()
            for k in range(KC):
                mm(c, p[:, 0:w], w2[:, k, m * 128:(m + 1) * 128], S[:, k, n0:n0 + w], k == 0, k == KC - 1,
                   [bw2, bS[nt]], [bp])
            tmp, btmp, _ = tr.next()
            if bias_row is not None:
                act(c, tmp[:, 0:w], p[:, 0:w], AF.Identity, [bp, self.bP, bbg], [btmp],
                    scale=self.modT[:, i, gpart, m, x:x + 1], bias=bgc[:, m, x:x + 1])
            else:
                act(c, tmp[:, 0:w], p[:, 0:w], AF.Identity, [bp, self.bP], [btmp],
                    scale=self.modT[:, i, gpart, m, x:x + 1])
            c.op("dve", lambda e: e.tensor_tensor(out=ht[:, 0:w], in0=ht[:, 0:w], in1=tmp[:, 0:w], op=ALU.add),
                 [bh, btmp], [bh])
            c.dma("sp", sst, [(A["Hd"][m, :, n0:n0 + w], ht[:, 0:w])], [bh], [bh])

    def stage_hgrn(self, i):
        c, A = self.c, self.A
        j = i // 2
        which = 0 if i == 1 else 1
        last = (i == DEPTH - 1)
        NB = NTOK // 128
        NCH = NTOK // GC
        LNQ = float(-0.5 * np.log(128.0))
        with c.stage():
            self.eps_const()
            Aact = c.sbuf("A", [128, KC, NTOK], BF16)
            bA = [c.buf() for _ in NT5]
            rstm = c.sbuf("rstm", [128, NTOK], F32)
            mF = c.sbuf("mF", [128, 2, 128], F32)
            lnq = c.sbuf("lnq", [128, 1], F32)
            bC = c.buf()
            sC = c.dsem()
            c.dma("sp", sC, [(rstm[:], A["consts"][:, C_RST:C_END]),
                             (mF[:, 0, :], A["consts"][:, C_MF:C_MF + 128]),
                             (mF[:, 1, :], A["consts"][:, C_MB:C_MB + 128])], [self.bIn], [bC])
            c.op("dve", lambda e: e.memset(lnq[:], LNQ), [], [bC])
            with c.stage():
                hr = Rot(c, "ch", [128, KC, 512], F32, 2, dma=True)
                tmp_rot = Rot(c, "ntmp", [128, 512], F32, 2)
                sq_rot = Rot(c, "nsq", [128, KC, 512], BF16, 1)
                rs_rot = Rot(c, "nrs", [128, 512], F32, 1)
                ps_n = Rot(c, "psn", [128, 512], F32, 2, "psum")
                for nt, (n0, w) in enumerate(NT5):
                    x = 0 if n0 < NLAT else 1
                    ht, bh, sh = hr.next()
                    c.dma("sp", sh, [(ht[:, :, 0:w], A["Hd"][:, :, n0:n0 + w].rearrange("k p t -> p k t"))],
                          [self.bIn], [bh])
                    self.norm_tile(lambda k: ht[:, k, 0:w], [bh], w,
                                   lambda k: self.nmT[:, i, 0, k, x:x + 1], lambda k: self.modT[:, i, 0, k, x:x + 1],
                                   lambda k: Aact[:, k, n0:n0 + w], [bA[nt]], tmp_rot, sq_rot, ps_n, rs_rot)
            with c.stage():
                whr = Rot(c, "wh", [128, 5, KC, 128], BF16, 2, dma=True)
                lf = [c.sbuf("lf", [128, NTOK], F32) for _ in range(2)]
                kk = [c.sbuf("kk", [128, NTOK], BF16) for _ in range(2)]
                qs = c.sbuf("qs", [128, NTOK], BF16)
                gs2 = [c.sbuf("gs", [128, NTOK], BF16) for _ in range(2)]
                vtok2 = [c.sbuf("vtok", [128, NB, 128], BF16) for _ in range(2)]
                Rh = Rot(c, "Rh", [128, NTOK], BF16, 2, dma=True)
                bc = c.sbuf("bc", [128, NTOK], F32)
                qin = [c.sbuf("qin", [128, NTOK], BF16) for _ in range(2)]
                kin = [c.sbuf("kin", [128, NTOK], BF16) for _ in range(2)]
                kstT = c.sbuf("kstT", [128, NTOK], BF16)
                ksttok = [c.sbuf("ksttok", [128, NB, 128], BF16) for _ in range(2)]
                kst3 = [c.sbuf("kst3", [128, NB, 128], BF16) for _ in range(2)]
                decs = [c.sbuf("decs", [128, NCH], F32) for _ in range(2)]
                oT = c.sbuf("oT", [128, NTOK], F32)
                s32r = [Rot(c, "S32", [128, 128], F32, 3) for _ in range(2)]
                sbr = [Rot(c, "Sb", [128, 128], BF16, 3) for _ in range(2)]
                tr = Rot(c, "etmp", [128, 512], F32, 3)
                amr = [Rot(c, "attm", [128, 128], BF16, 2) for _ in range(2)]
                sqr = Rot(c, "osq", [128, 512], BF16, 1)
                rsr = Rot(c, "ors", [128, 512], F32, 1)
                pg = Rot(c, "pg", [128, 512], F32, 2, "psum")
                pT = Rot(c, "pT", [128, 1024], BF16, 1, "psum")
                pa = Rot(c, "pa", [128, 512], F32, 1, "psum")
                po = Rot(c, "po", [128, 512], F32, 2, "psum")
                pk = Rot(c, "pk", [128, 512], F32, 2, "psum")
                Win = A["rec_w_in"][j]
                blf = [[c.buf() for _ in NT5] for _ in range(2)]
                bkk = [[c.buf() for _ in NT5] for _ in range(2)]
                bqs = [c.buf() for _ in NT5]
                bgs2 = [[c.buf() for _ in NT5] for _ in range(2)]
                bvt2 = [c.buf(), c.buf()]
                bo = [c.buf() for _ in range(NB)]
                bbc = c.buf()
                bqi = [[c.buf() for _ in NT5] for _ in range(2)]
                bki = [[c.buf() for _ in NT5] for _ in range(2)]
                bks = [c.buf() for _ in NT5]
                bdec = [c.buf(), c.buf()]
                bkt = [c.buf(), c.buf()]
                pgp, bpgp = pg.t[1], pg.b[1]

                def proj_thunks(h):
                    hb = h % 2
                    st = {}
                    th = []

                    def t_load():
                        wh, bwh, swh = whr.next()
                        st.update(wh=wh, bwh=bwh)
                        c.dma("pool", swh, [(wh[:, part], Win[:, part * D + h * 128:part * D + (h + 1) * 128]
                                             .rearrange("(k p) c -> p k c", p=128)) for part in range(5)],
                              [self.bIn], [bwh])
                    th.append(t_load)

                    def vgroup(g):
                        def run():
                            wh, bwh = st["wh"], st["bwh"]
                            nb_ = min(4, NB - g * 4)
                            for q in range(nb_):
                                blk = g * 4 + q
                                for k in range(KC):
                                    mm(c, pgp[:, q * 128:(q + 1) * 128], Aact[:, k, blk * 128:(blk + 1) * 128],
                                       wh[:, 0, k, :], k == 0, k == KC - 1, [bwh] + bA, [bpgp])
                            c.op("act", lambda e: e.copy(out=vtok2[hb][:, g * 4:g * 4 + nb_, :],
                                                         in_=pgp[:, 0:nb_ * 128].rearrange("p (a b) -> p a b", b=128)),
                                 [bpgp], [bvt2[hb]])
                        return run
                    for g in range((NB + 3) // 4):
                        th.append(vgroup(g))

                    def zgroup(part, nt):
                        def run():
                            wh, bwh = st["wh"], st["bwh"]
                            n0, w = NT5[nt]
                            for k in range(KC):
                                mm(c, pgp[:, 0:w], wh[:, part, k, :], Aact[:, k, n0:n0 + w], k == 0, k == KC - 1,
                                   [bwh, bA[nt]], [bpgp])
                            if part in (1, 2):
                                act(c, lf[part - 1][:, n0:n0 + w], pgp[:, 0:w], AF.Sigmoid, [bpgp],
                                    [blf[part - 1][nt]])
                            elif part == 3:
                                act(c, qs[:, n0:n0 + w], pgp[:, 0:w], AF.Silu, [bpgp], [bqs[nt]])
                            else:
                                act(c, gs2[hb][:, n0:n0 + w], pgp[:, 0:w], AF.Silu, [bpgp], [bgs2[hb][nt]])
                        return run

                    def fk():
                        for d in range(2):
                            c.op("dve", lambda e: e.tensor_scalar(
                                out=lf[d][:], in0=lf[d][:], scalar1=self.lbT[:, h, d, which, 1:2],
                                scalar2=self.lbT[:, h, d, which, 0:1], op0=ALU.mult, op1=ALU.add),
                                blf[d] + [self.bP], blf[d])
                            c.op("dve", lambda e: e.tensor_scalar(
                                out=kk[d][:], in0=lf[d][:], scalar1=-1.0, scalar2=1.0,
                                op0=ALU.mult, op1=ALU.add), blf[d], bkk[d])
                        for d in range(2):
                            act(c, lf[d][:], lf[d][:], AF.Ln, blf[d], blf[d])
                    for part in (1, 2, 3, 4):
                        for nt in range(len(NT5)):
                            th.append(zgroup(part, nt))
                        if part == 2:
                            th.append(fk)
                    return th

                for th_ in proj_thunks(0):
                    th_()
                kvbanks = [(pk.t[0], pk.b[0]), (pk.t[1], pk.b[1]), (pg.t[0], pg.b[0]),
                           (pT.t[0][:].bitcast(F32), pT.b[0])]
                for h in range(KC):
                    hb = h % 2
                    nxt = proj_thunks(h + 1) if h + 1 < KC else []
                    for d in range(2):
                        c.op("dve", lambda e: e.tensor_tensor_scan(out=bc[:], data0=rstm[:], data1=lf[d][:],
                                                                   initial=0.0, op0=ALU.mult, op1=ALU.add),
                             blf[d] + [bC], [bbc])
                        for nt, (n0, w) in enumerate(NT5):
                            nc_ = w // GC
                            bc3 = bc[:, n0:n0 + w].rearrange("p (a b) -> p a b", b=GC)
                            if d == 1:
                                t0, bt0, _ = tr.next()
                                c.op("dve", lambda e: e.tensor_tensor(
                                    out=t0[:, 0:w].rearrange("p (a b) -> p a b", b=GC),
                                    in0=bc3[:, :, GC - 1:GC].broadcast_to([128, nc_, GC]), in1=bc3,
                                    op=ALU.subtract), [bbc], [bt0])
                                c.op("dve", lambda e: e.tensor_tensor(out=bc[:, n0:n0 + w], in0=t0[:, 0:w],
                                                                      in1=lf[d][:, n0:n0 + w], op=ALU.add),
                                     [bt0, blf[d][nt]], [bbc])
                            tot = bc3[:, :, GC - 1:GC] if d == 0 else bc3[:, :, 0:1]
                            t1, bt1, _ = tr.next()
                            act(c, t1[:, 0:w], bc[:, n0:n0 + w], AF.Exp, [bbc, bC], [bt1], bias=lnq[:, 0:1])
                            c.op("dve", lambda e: e.tensor_tensor(out=qin[d][:, n0:n0 + w], in0=qs[:, n0:n0 + w],
                                                                  in1=t1[:, 0:w], op=ALU.mult),
                                 [bt1, bqs[nt]], [bqi[d][nt]])
                            t2, bt2, _ = tr.next()
                            act(c, t2[:, 0:w], bc[:, n0:n0 + w], AF.Exp, [bbc], [bt2], scale=-1.0)
                            c.op("pool", lambda e: e.tensor_tensor(out=kin[d][:, n0:n0 + w],
                                                                   in0=kk[d][:, n0:n0 + w],
                                                                   in1=t2[:, 0:w], op=ALU.mult),
                                 [bt2, bkk[d][nt]], [bki[d][nt]])
                            t3, bt3, _ = tr.next()
                            c.op("dve", lambda e: e.tensor_tensor(
                                out=t3[:, 0:w].rearrange("p (a b) -> p a b", b=GC),
                                in0=tot.broadcast_to([128, nc_, GC]), in1=bc3, op=ALU.subtract), [bbc], [bt3])
                            act(c, t3[:, 0:w], t3[:, 0:w], AF.Exp, [bt3], [bt3])
                            c.op("pool" if nt % 2 == 0 else "dve",
                                 lambda e: e.tensor_tensor(out=kstT[:, n0:n0 + w], in0=kk[d][:, n0:n0 + w],
                                                           in1=t3[:, 0:w], op=ALU.mult),
                                 [bt3, bkk[d][nt]], [bks[nt]])
                            act(c, decs[d][:, n0 // GC:n0 // GC + nc_], tot, AF.Exp, [bbc], [bdec[d]])
                        for g in range((NB + 3) // 4):
                            p, bp, _ = pT.next()
                            nb_ = min(4, NB - g * 4)
                            for q in range(nb_):
                                blk = g * 4 + q
                                c.op("pe", lambda e: e.transpose(out=p[:, q * 128:(q + 1) * 128],
                                                                 in_=kstT[:, blk * 128:(blk + 1) * 128],
                                                                 identity=self.identb[:]), bks + [self.bP], [bp])
                            c.op("dve", lambda e: e.tensor_copy(
                                out=ksttok[d][:, g * 4:g * 4 + nb_, :],
                                in_=p[:, 0:nb_ * 128].rearrange("p (a b) -> p a b", b=128)), [bp], [bkt[d]])
                            c.op("dve", lambda e: e.tensor_scalar(
                                out=kst3[d][:, g * 4:g * 4 + nb_, :],
                                in0=p[:, 0:nb_ * 128].rearrange("p (a b) -> p a b", b=128),
                                scalar1=mF[:, 0, 127:128], scalar2=None, op0=ALU.mult), [bp, bC], [bkt[d]])
                    orders = [[16, 17] + list(range(16)), [17, 16] + list(range(15, -1, -1))]
                    chl = [[0, 1, 2, 3], [3, 2, 1, 0]]
                    chunks = [[(blk, cc) for blk in orders[d] for cc in chl[d]] for d in range(2)]
                    ST = []
                    for d in range(2):
                        S32, bS32, _ = s32r[d].next()
                        c.op("dve", lambda e: e.memset(S32[:], 0.0), [], [bS32])
                        sb, bsb, _ = sbr[d].next()
                        c.op("pool", lambda e: e.memset(sb[:], 0.0), [], [bsb])
                        ST.append(dict(S32=S32, bS32=bS32, sb=sb, bsb=bsb, kvi=0))

                    def att(d, blk):
                        cols = slice(blk * 128, (blk + 1) * 128)
                        nt = min(blk // 4, 4)
                        pA, bpA, _ = pa.next()
                        mm(c, pA[:, 0:128], kin[d][:, cols], qin[d][:, cols], True, True,
                           [bki[d][nt], bqi[d][nt]], [bpA])
                        am, bam, _ = amr[d].next()
                        c.op("dve", lambda e: e.tensor_tensor(out=am[:], in0=pA[:, 0:128], in1=mF[:, d, :],
                                                              op=ALU.mult), [bpA, bC], [bam])
                        return am, bam

                    def kv(d, blk, cc):
                        pK, bpK = kvbanks[2 * d + ST[d]["kvi"] % 2]
                        ST[d]["kvi"] += 1
                        if cc < 3:
                            mm(c, pK[:, 0:128], ksttok[d][cc * GC:(cc + 1) * GC, blk, :],
                               vtok2[hb][cc * GC:(cc + 1) * GC, blk, :], True, True, [bkt[d], bvt2[hb]], [bpK])
                        else:
                            mm(c, pK[:, 0:128], kst3[d][64:128, blk, :], vtok2[hb][64:128, blk, :], True, True,
                               [bkt[d], bvt2[hb]], [bpK])
                        return pK, bpK

                    for d in range(2):
                        ST[d]["att"] = att(d, orders[d][0])
                        ST[d]["kv"] = kv(d, *chunks[d][0])
                    owritten = set()
                    for idx in range(len(chunks[0])):
                        if nxt and idx >= 2 and idx % 2 == 0:
                            nxt.pop(0)()
                        for d in range(2):
                            X = ST[d]
                            blk, cc = chunks[d][idx]
                            cols = slice(blk * 128, (blk + 1) * 128)
                            nt = min(blk // 4, 4)
                            ci = idx % 4
                            if ci == 0:
                                am, bam = X["att"]
                                oi = idx // 4
                                if oi + 1 < NB:
                                    X["att"] = att(d, orders[d][oi + 1])
                                pO, bpO = po.t[d], po.b[d]
                                mm(c, pO[:, 0:128], vtok2[hb][:, blk, :], am[:], True, False, [bvt2[hb], bam], [bpO])
                            pO, bpO = po.t[d], po.b[d]
                            pK, bpK = X["kv"]
                            if idx + 1 < len(chunks[d]):
                                X["kv"] = kv(d, *chunks[d][idx + 1])
                            ch = blk * 4 + cc
                            mm(c, pO[:, cc * GC:(cc + 1) * GC], X["sb"][:], qin[d][:, ch * GC:(ch + 1) * GC],
                               False, ci == 3, [X["bsb"], bqi[d][nt]], [bpO])
                            S32n, bS32n, _ = s32r[d].next()
                            S32o, bS32o = X["S32"], X["bS32"]
                            c.op("dve", lambda e: e.scalar_tensor_tensor(
                                out=S32n[:], in0=S32o[:], scalar=decs[d][:, ch:ch + 1], in1=pK[:, 0:128],
                                op0=ALU.mult, op1=ALU.add), [bS32o, bdec[d], bpK], [bS32n])
                            X["S32"], X["bS32"] = S32n, bS32n
                            sb, bsb, _ = sbr[d].next()
                            c.op("act", lambda e: e.copy(out=sb[:], in_=S32n[:]), [bS32n], [bsb])
                            X["sb"], X["bsb"] = sb, bsb
                            if ci == 3:
                                if blk not in owritten:
                                    owritten.add(blk)
                                    c.op("act", lambda e: e.copy(out=oT[:, cols], in_=pO[:, 0:128]), [bpO], [bo[blk]])
                                else:
                                    c.op("dve", lambda e: e.tensor_tensor(out=oT[:, cols], in0=oT[:, cols],
                                                                          in1=pO[:, 0:128], op=ALU.add),
                                         [bpO, bo[blk]], [bo[blk]])
                    while nxt:
                        nxt.pop(0)()
                    rh, brh, srh = Rh.next()
                    for nt, (n0, w) in enumerate(NT5):
                        bos = bo[n0 // 128:(n0 + w) // 128]
                        sq, bsq, _ = sqr.next()
                        c.op("pool", lambda e: e.tensor_tensor(out=sq[:, 0:w], in0=oT[:, n0:n0 + w],
                                                               in1=oT[:, n0:n0 + w], op=ALU.mult), bos, [bsq])
                        mm(c, pgp[:, 0:w], self.onesb[:], sq[:, 0:w], True, True, [bsq, self.bP], [bpgp])
                        rs, brs, _ = rsr.next()
                        act(c, rs[:, 0:w], pgp[:, 0:w], AF.Ln, [bpgp], [brs], scale=1.0 / 128, bias=self.epsc[:, 0:1])
                        act(c, rs[:, 0:w], rs[:, 0:w], AF.Exp, [brs], [brs], scale=-0.5)
                        c.op("dve", lambda e: e.tensor_tensor(out=rs[:, 0:w], in0=rs[:, 0:w], in1=oT[:, n0:n0 + w],
                                                              op=ALU.mult), [brs] + bos, [brs])
                        c.op("dve", lambda e: e.scalar_tensor_tensor(
                            out=rh[:, n0:n0 + w], in0=rs[:, 0:w], scalar=self.vcol(R_ONG + j, h),
                            in1=gs2[hb][:, n0:n0 + w], op0=ALU.mult, op1=ALU.mult),
                            [brs, bgs2[hb][nt], self.bP], [brh])
                    c.dma("sp", srh, [(A["Rd"][h], rh[:])], [brh], [brh])
            with c.stage():
                w2 = c.sbuf("wo", [128, KC, D], BF16)
                bw2 = c.buf()
                sw2 = c.dsem()
                c.dma("pool", sw2, [(w2[:], A["rec_w_o"][j].rearrange("(k p) m -> p k m", p=128))],
                      [self.bIn], [bw2])
                Rr = c.sbuf("R", [128, KC, NTOK], BF16)
                bR1 = c.buf()
                sR = c.dsem()
                c.dma("sp", sR, [(Rr[:], A["Rd"].rearrange("k p t -> p k t"))], [self.bIn], [bR1])
                self.residual_out(i, 2, w2, bw2, Rr, [bR1] * len(NT5), NT5[:4] if last else NT5)

def make_consts():
    cs = np.zeros((128, C_END), np.float32)
    cs[:, C_ID:C_ID + 128] = np.eye(128, dtype=np.float32)
    s = np.arange(128)[:, None]
    t = np.arange(128)[None, :]
    same = (s // GC) == (t // GC)
    cs[:, C_MF:C_MF + 128] = (same & (s <= t)).astype(np.float32)
    cs[:, C_MB:C_MB + 128] = (same & (s >= t)).astype(np.float32)
    rst = np.ones(NTOK, np.float32)
    rst[::GC] = 0.0
    cs[:, C_RST:C_END] = rst[None, :]
    return cs


def make_vecs(inp, b):
    v = np.zeros((128, D), np.float32)
    v[R_C] = inp["c"][b]
    v[R_CCTX] = inp["c_ctx"]
    v[R_ADAB:R_ADAB + 24] = inp["ada_b"].reshape(24, D)
    v[R_NMIX:R_NMIX + 4] = inp["norm_mix_g"]
    v[R_NFFN:R_NFFN + 4] = inp["norm_ffn_g"]
    v[R_FING] = inp["final_g"]
    v[R_PW1B:R_PW1B + 4] = inp["conv_pw1_b"].reshape(4, D)
    v[R_DWW:R_DWW + 62] = inp["conv_dw_w"].reshape(62, D)
    v[R_DWB:R_DWB + 2] = inp["conv_dw_b"]
    v[R_LNG:R_LNG + 2] = inp["conv_ln_g"]
    v[R_LNB:R_LNB + 2] = inp["conv_ln_b"]
    v[R_PW2B:R_PW2B + 2] = inp["conv_pw2_b"]
    v[R_LBL:R_LBL + 8] = inp["rec_lb_logits"].reshape(8, D)
    v[R_ONG:R_ONG + 2] = inp["rec_onorm_g"]
    return v


WKEYS = ["ada_w", "conv_pw1_w", "conv_pw2_w", "rec_w_in", "rec_w_o", "ffn_w1", "ffn_w3", "ffn_w2",
         "moe_router", "moe_w1", "moe_w3", "moe_w2"]


def make_in_maps(inp, cores, used=None):
    consts = make_consts()
    shared = {k: np.ascontiguousarray(inp[k], dtype=np.float32) for k in WKEYS if used is None or k in used}
    maps = []
    for b in cores:
        m = dict(shared)
        m["xin"] = np.ascontiguousarray(np.concatenate([inp["x"][b], inp["ctx"][b]], axis=0), dtype=np.float32)
        m["vecs"] = make_vecs(inp, b)
        m["consts"] = consts
        maps.append(m)
    return maps


_CACHE = {}


def kernel(**inputs):
    inp = {k: np.asarray(v) for k, v in inputs.items()}
    if "nc" not in _CACHE:
        _CACHE["prog"] = Prog()
        _CACHE["nc"] = _CACHE["prog"].build()
    nc = _CACHE["nc"]
    maps = make_in_maps(inp, list(range(8)), set(_CACHE["prog"].A.keys()))
    res = run_bass_kernel_spmd(nc, maps, core_ids=list(range(8)))
    out = np.stack([np.asarray(r["out"]) for r in res.results], axis=0)
    return out.astype(np.float32)
```

```python
from contextlib import ExitStack, nullcontext
import numpy as np
import concourse.bass as bass
import concourse.mybir as mybir
from concourse.bass_utils import run_bass_kernel_spmd

F32 = mybir.dt.float32
BF16 = mybir.dt.bfloat16
AF = mybir.ActivationFunctionType
ALU = mybir.AluOpType
AX = mybir.AxisListType

NTOK, NLAT, NCTX, D, KC, FF, FC, NE = 2304, 2048, 256, 1024, 8, 2816, 22, 8
DEPTH = 4
EPS = 1e-6
CW = 31
GC = 32
R_C, R_CCTX, R_ADAB, R_NMIX, R_NFFN, R_FING, R_PW1B, R_DWW, R_DWB, R_LNG, R_LNB, R_PW2B, R_LBL, R_ONG = \
    0, 1, 2, 26, 30, 34, 35, 39, 101, 103, 105, 107, 109, 117
C_ID, C_MF, C_MB, C_RST, C_END = 0, 128, 256, 384, 384 + NTOK

NT5 = [(0, 512), (512, 512), (1024, 512), (1536, 512), (2048, 256)]
ST3 = [[(0, 384), (384, 384)], [(768, 384), (1152, 384)], [(1536, 512), (2048, 256)]]


class Buf:
    __slots__ = ("name", "writers", "readers")

    def __init__(self, name):
        self.name = name
        self.writers = {}
        self.readers = {}


class DSem:
    __slots__ = ("key",)

    def __init__(self, key):
        self.key = key


class Ctx:
    ENG = ("pe", "dve", "act", "pool", "sp")
    NPOOL = 90
    NSW = 20

    def __init__(self, nc, stack):
        self.nc = nc
        self.top = stack
        self.stack = stack
        self.eng = {"pe": nc.tensor, "dve": nc.vector, "act": nc.scalar,
                    "pool": nc.gpsimd, "sp": nc.sync}
        self.sems = {}
        self.cnt = {}
        for e in self.ENG:
            self.sems[e] = stack.enter_context(nc.semaphore("c_" + e))
            self.cnt[e] = 0
        self.free_keys = {True: [], False: []}
        for i in range(self.NPOOL):
            k = i
            self.sems[k] = stack.enter_context(nc.semaphore("d%d" % i))
            self.cnt[k] = 0
            self.free_keys[i < self.NSW].append(k)
        self.key_sw = {}
        self.stage_keys = []
        self.waited = {}
        self.uid = 0
        self.n_inst = {e: 0 for e in self.ENG}

    def sbuf(self, name, shape, dt):
        self.uid += 1
        return self.stack.enter_context(self.nc.sbuf_tensor("%s_%d" % (name, self.uid), list(shape), dt))

    def psum(self, name, shape=(128, 512), dt=F32):
        self.uid += 1
        return self.stack.enter_context(self.nc.psum_tensor("%s_%d" % (name, self.uid), list(shape), dt))

    def buf(self, name="b"):
        self.uid += 1
        return Buf("%s%d" % (name, self.uid))

    def bufs(self, n, name="b"):
        return [self.buf(name) for _ in range(n)]

    def dsem(self):
        return DSem(None)

    def _bind(self, sem, q):
        if sem.key is None:
            sw = (q == "pool")
            k = self.free_keys[sw].pop()
            self.key_sw[k] = sw
            self.stage_keys.append(k)
            sem.key = k
        return sem.key

    def _wait(self, e, key, val):
        if val <= 0:
            return
        k = (e, key)
        if self.waited.get(k, 0) >= val:
            return
        if not isinstance(key, str):
            val = max(val, self.cnt[key])
        self.waited[k] = val
        self.eng[e].wait_ge(self.sems[key], val)

    def _need(self, e, reads, writes, same_raw, is_dma=False):
        need = {}
        for b in reads:
            for k, v in b.writers.items():
                if k == e and not same_raw:
                    continue
                need[k] = max(need.get(k, 0), v)
        for b in writes:
            for k, v in b.writers.items():
                if k == e and e == "pe":
                    continue
                need[k] = max(need.get(k, 0), v)
            for k, v in b.readers.items():
                if k == e and e == "pe":
                    continue
                need[k] = max(need.get(k, 0), v)
        for k, v in need.items():
            self._wait(e, k, v)

    def op(self, e, fn, reads=(), writes=(), same_raw=True):
        self._need(e, reads, writes, same_raw)
        inst = fn(self.eng[e])
        self.cnt[e] += 1
        self.n_inst[e] += 1
        inst.then_inc(self.sems[e], 1)
        v = self.cnt[e]
        for b in reads:
            b.readers[e] = v
        for b in writes:
            b.writers = {e: v}
            b.readers = {}
        return inst

    def dma(self, q, sem, pairs, reads, writes, **kw):
        key = self._bind(sem, q)
        self._need(q, reads, writes, True, is_dma=True)
        for (o, i) in pairs:
            self.eng[q].dma_start(out=o, in_=i, **kw).then_inc(self.sems[key], 16)
            self.cnt[key] += 16
            self.n_inst[q] += 1
        v = self.cnt[key]
        for r in reads:
            r.readers[key] = v
        for b in writes:
            b.writers = {key: v}
            b.readers = {}

    def barrier(self):
        keys = list(self.ENG) + list(self.stage_keys)
        for e in self.ENG:
            for k in keys:
                self._wait(e, k, self.cnt[k])

    class _Stage:
        def __init__(self, c):
            self.c = c

        def __enter__(self):
            c = self.c
            self.prev = c.stack
            self.es = ExitStack()
            self.es.__enter__()
            c.stack = self.es
            self.keys0 = len(c.stage_keys)
            return c

        def __exit__(self, *a):
            c = self.c
            c.barrier()
            rel = c.stage_keys[self.keys0:]
            del c.stage_keys[self.keys0:]
            for k in rel:
                c.free_keys[c.key_sw[k]].append(k)
            c.stack = self.prev
            return self.es.__exit__(*a)

    def stage(self):
        return Ctx._Stage(self)


class Rot:
    def __init__(self, c, name, shape, dt, n, kind="sbuf", dma=False):
        self.t = []
        self.b = []
        self.s = []
        for i in range(n):
            self.t.append(c.sbuf(name, shape, dt) if kind == "sbuf" else c.psum(name, shape, dt))
            self.b.append(c.buf(name))
            self.s.append(c.dsem() if dma else None)
        self.i = 0
        self.n = n

    def next(self):
        i = self.i
        self.i = (i + 1) % self.n
        return self.t[i], self.b[i], self.s[i]


def act(c, out, in_, func, reads, writes, **kw):
    return c.op("act", lambda e: e.activation(out=out, in_=in_, func=func, **kw), reads, writes)


def mm(c, out, lhsT, rhs, start, stop, reads, writes):
    return c.op("pe", lambda e: e.matmul(out, lhsT=lhsT, rhs=rhs, start=start, stop=stop), reads, writes)


class Prog:
    def __init__(self, n_stage_list=None, dbg=None):
        self.stages = n_stage_list
        self.dbg = dbg

    def build(self):
        nc = bass.Bass("TRN2", target_bir_lowering=False)
        self.nc = nc
        dt = nc.dram_tensor
        prog = self

        class LazyA(dict):
            def __missing__(self, k):
                v = dt(k, prog.wshape[k], F32, kind="ExternalInput").ap()
                self[k] = v
                return v
        A = LazyA()
        A["xin"] = dt("xin", [NTOK, D], F32, kind="ExternalInput").ap()
        A["vecs"] = dt("vecs", [128, D], F32, kind="ExternalInput").ap()
        A["consts"] = dt("consts", [128, C_END], F32, kind="ExternalInput").ap()
        self.wshape = {"ada_w": [DEPTH, D, 6 * D], "conv_pw1_w": [2, D, 2 * D], "conv_pw2_w": [2, D, D],
                       "rec_w_in": [2, D, 5 * D], "rec_w_o": [2, D, D], "ffn_w1": [2, D, FF], "ffn_w3": [2, D, FF],
                       "ffn_w2": [2, FF, D], "moe_router": [2, D, NE], "moe_w1": [2, NE, D, FF],
                       "moe_w3": [2, NE, D, FF], "moe_w2": [2, NE, FF, D]}
        A["out"] = dt("out", [NLAT, D], F32, kind="ExternalOutput").ap()
        A["Hd"] = dt("Hd", [KC, 128, NTOK], F32, kind="Internal").ap()
        A["Rd"] = dt("Rd", [KC, 128, NTOK], BF16, kind="Internal").ap()
        self.A = A
        with ExitStack() as top:
            c = Ctx(nc, top)
            self.c = c
            self.bHd = c.buf("Hd")
            self.bOut = c.buf("out")
            self.bIn = c.buf("in")
            self.persistent()
            stages = self.stages or (["setup", "input"] +
                                     sum([["mix%d" % i, "ffn%d" % i] for i in range(DEPTH)], []) + ["final"])
            for s in stages:
                if s == "setup":
                    self.stage_setup()
                elif s == "input":
                    if self.stages:
                        self.stage_input()
                elif s.startswith("mix"):
                    i = int(s[3:])
                    if i % 2 == 0:
                        self.stage_conv(i)
                    else:
                        self.stage_hgrn(i)
                elif s.startswith("ffn"):
                    i = int(s[3:])
                    self.stage_ffn(i, moe=(i % 2 == 1))
                elif s == "final":
                    self.stage_final()
                elif s == "dump":
                    self.stage_dump()
                elif s == "dumpctx":
                    self.stage_dump(ctx=True)
            c.barrier()
            for k, v in self.bOut.writers.items():
                c._wait("sp", k, v)
        return nc

    def persistent(self):
        c = self.c
        self.vT = c.sbuf("vT", [128, KC, 128], F32)
        self.modT = c.sbuf("modT", [128, DEPTH, 6, KC, 2], F32)
        self.nmT = c.sbuf("nmT", [128, DEPTH, 2, KC, 2], F32)
        self.lbT = c.sbuf("lbT", [128, KC, 2, 2, 2], F32)
        self.identf = c.sbuf("identf", [128, 128], F32)
        self.identb = c.sbuf("identb", [128, 128], BF16)
        self.onesb = c.sbuf("onesb", [128, 128], BF16)
        self.scb = c.sbuf("scb", [128, KC, 2], BF16)
        self.bP = c.buf("persist")

    def vcol(self, r, k):
        return self.vT[:, k, r:r + 1]

    def stage_setup(self):
        c, A = self.c, self.A
        with c.stage():
            vs = c.sbuf("vs", [128, D], F32)
            bvs = c.buf()
            s0 = c.dsem()
            c.dma("sp", s0, [(vs[:], A["vecs"]), (self.identf[:], A["consts"][:, C_ID:C_ID + 128])],
                  [self.bIn], [bvs])
            c.op("dve", lambda e: e.tensor_copy(out=self.identb[:], in_=self.identf[:]), [bvs], [self.bP])
            c.op("dve", lambda e: e.memset(self.onesb[:], 1.0), [], [self.bP])
            pst = Rot(c, "pst", [128, 512], F32, 2, "psum")
            for k in range(KC):
                ps, bps, _ = pst.next()
                c.op("pe", lambda e: e.transpose(out=ps[:, 0:128], in_=vs[:, k * 128:(k + 1) * 128],
                                                 identity=self.identf[:]), [bvs, self.bP], [bps])
                c.op("dve", lambda e: e.tensor_copy(out=self.vT[:, k, :], in_=ps[:, 0:128]), [bps], [self.bP])
            act(c, self.scb[:], self.vT[:, :, R_C:R_C + 2], AF.Silu, [self.bP], [self.bP])
            if not self.stages:
                self.stage_input(own_stage=False)
            mps = Rot(c, "mps", [128, 512], F32, 2, "psum")
            for li in (range(DEPTH) if self.stages else range(1)):
                for th in self.adaln_pieces(li, mps):
                    th()
            ex = c.sbuf("ex", [128, KC, 8], F32)
            bex = c.buf()
            act(c, ex[:], self.vT[:, :, R_LBL:R_LBL + 8], AF.Exp, [self.bP], [bex])
            sm = c.sbuf("sm", [128, KC, 2], F32)
            for d in range(2):
                c.op("dve", lambda e: e.tensor_reduce(out=sm[:, :, d], in_=ex[:, :, d * 4:(d + 1) * 4],
                                                      axis=AX.X, op=ALU.add), [bex], [bex])
            c.op("dve", lambda e: e.reciprocal(out=sm[:], in_=sm[:]), [bex], [bex])
            for d in range(2):
                c.op("dve", lambda e: e.tensor_tensor(out=self.lbT[:, :, d, 0, 0], in0=ex[:, :, d * 4 + 1],
                                                      in1=sm[:, :, d], op=ALU.mult), [bex], [self.bP])
                c.op("dve", lambda e: e.tensor_tensor(out=self.lbT[:, :, d, 1, 1], in0=ex[:, :, d * 4 + 0],
                                                      in1=sm[:, :, d], op=ALU.mult), [bex], [self.bP])
                c.op("dve", lambda e: e.tensor_scalar(out=self.lbT[:, :, d, 0, 1], in0=self.lbT[:, :, d, 0, 0],
                                                      scalar1=-1.0, scalar2=1.0, op0=ALU.mult, op1=ALU.add),
                     [self.bP], [self.bP])
                c.op("dve", lambda e: e.tensor_scalar(out=self.lbT[:, :, d, 1, 0], in0=self.lbT[:, :, d, 1, 1],
                                                      scalar1=-1.0, scalar2=1.0, op0=ALU.mult, op1=ALU.add),
                     [self.bP], [self.bP])

    def adaln_pieces(self, i, mps):
        c, A = self.c, self.A
        wrot = Rot(c, "adaw", [128, 3, 1024], BF16, 2, dma=True)
        acc = c.sbuf("acc", [128, 48, 2], F32)
        bacc = c.buf()
        out = []

        def piece(k, half):
            def run():
                wt, bw, sw = wrot.next()
                src = A["ada_w"][i, k * 128:(k + 1) * 128, half * 3072:(half + 1) * 3072]
                c.dma("pool", sw, [(wt[:], src.rearrange("p (a b) -> p a b", b=1024))], [self.bIn], [bw])
                ps, bps, _ = mps.next()
                psv = ps[:, 0:48].rearrange("p (a b) -> p a b", b=2)
                for ec in range(24):
                    a_, b_ = divmod(ec * 128, 1024)
                    mm(c, psv[:, ec, :], wt[:, a_, b_:b_ + 128], self.scb[:, k, :], True, True,
                       [bw, self.bP], [bps])
                dst = acc[:, half * 24:(half + 1) * 24, :]
                if k == 0:
                    c.op("dve", lambda e: e.tensor_copy(out=dst, in_=psv[:, 0:24, :]), [bps], [bacc])
                else:
                    c.op("dve", lambda e: e.tensor_tensor(out=dst, in0=dst, in1=psv[:, 0:24, :], op=ALU.add),
                         [bps, bacc], [bacc])
            return run

        for k in range(KC):
            for half in range(2):
                out.append(piece(k, half))

        def fin():
            for part in range(6):
                for x in range(2):
                    c.op("dve", lambda e: e.tensor_tensor(
                        out=self.modT[:, i, part, :, x], in0=acc[:, part * 8:(part + 1) * 8, x],
                        in1=self.vT[:, :, R_ADAB + i * 6 + part], op=ALU.add), [bacc, self.bP], [self.bP])
            for which, (rg, part) in enumerate(((R_NMIX + i, 1), (R_NFFN + i, 4))):
                for x in range(2):
                    c.op("dve", lambda e: e.scalar_tensor_tensor(
                        out=self.nmT[:, i, which, :, x], in0=self.modT[:, i, part, :, x], scalar=1.0,
                        in1=self.vT[:, :, rg], op0=ALU.add, op1=ALU.mult), [self.bP], [self.bP])
        out.append(fin)
        return out

    def stage_input(self, own_stage=True):
        c, A = self.c, self.A
        with (c.stage() if own_stage else nullcontext()):
            xr = Rot(c, "xin", [128, D], F32, 3, dma=True)
            pr = Rot(c, "pin", [128, 512], F32, 4, "psum")
            hr = Rot(c, "hin", [128, KC, 128], F32, 3, dma=True)
            for tb in range(NTOK // 128):
                xt, bx, sx = xr.next()
                c.dma("sp", sx, [(xt[:], A["xin"][tb * 128:(tb + 1) * 128, :])], [self.bIn], [bx])
                ht, bh, sh = hr.next()
                for g in range(2):
                    ps, bps, _ = pr.next()
                    for q in range(4):
                        k = g * 4 + q
                        c.op("pe", lambda e: e.transpose(out=ps[:, q * 128:(q + 1) * 128],
                                                         in_=xt[:, k * 128:(k + 1) * 128],
                                                         identity=self.identf[:]), [bx, self.bP], [bps])
                    eng = "dve" if g == 0 else "act"
                    dst = ht[:, g * 4:(g + 1) * 4, :]
                    src = ps[:].rearrange("p (a b) -> p a b", b=128)
                    if eng == "dve":
                        c.op("dve", lambda e: e.tensor_copy(out=dst, in_=src), [bps], [bh])
                    else:
                        c.op("act", lambda e: e.copy(out=dst, in_=src), [bps], [bh])
                c.dma("sp", sh, [(A["Hd"][:, :, tb * 128:(tb + 1) * 128].rearrange("k p t -> p k t"), ht[:])],
                      [bh], [c.buf()])

    def norm_tile(self, ht, bh, w, nm, sh, out, bout, tmp_rot, sq_rot, ps_rot, rs_rot, out32=None):
        c = self.c
        sq, bsq, _ = sq_rot.next()
        for k in range(KC):
            act(c, sq[:, k, 0:w], ht(k), AF.Square, bh, [bsq])
        ps, bps, _ = ps_rot.next()
        for k in range(KC):
            mm(c, ps[:, 0:w], self.onesb[:], sq[:, k, 0:w], k == 0, k == KC - 1, [bsq, self.bP], [bps])
        rs, brs, _ = rs_rot.next()
        act(c, rs[:, 0:w], ps[:, 0:w], AF.Ln, [bps], [brs], scale=1.0 / D, bias=self.epsc[:, 0:1])
        act(c, rs[:, 0:w], rs[:, 0:w], AF.Exp, [brs], [brs], scale=-0.5)
        for k in range(KC):
            tmp, btmp, _ = tmp_rot.next()
            c.op("dve", lambda e: e.tensor_tensor(out=tmp[:, 0:w], in0=ht(k), in1=rs[:, 0:w], op=ALU.mult),
                 list(bh) + [brs], [btmp])
            if sh is not None:
                c.op("dve", lambda e: e.tensor_scalar(out=out(k), in0=tmp[:, 0:w], scalar1=nm(k), scalar2=sh(k),
                                                      op0=ALU.mult, op1=ALU.add), [btmp, self.bP], bout)
            else:
                c.op("dve", lambda e: e.tensor_scalar(out=out(k), in0=tmp[:, 0:w], scalar1=nm(k), scalar2=None,
                                                      op0=ALU.mult), [btmp, self.bP], bout)
            if out32 is not None:
                c.op("pool", lambda e: e.tensor_scalar(out=out32(k), in0=tmp[:, 0:w], scalar1=nm(k), scalar2=sh(k),
                                                       op0=ALU.mult, op1=ALU.add), [btmp, self.bP], bout)

    def eps_const(self):
        c = self.c
        self.epsc = c.sbuf("epsc", [128, 1], F32)
        c.op("dve", lambda e: e.memset(self.epsc[:], EPS), [], [self.bP])

    def stage_ffn(self, i, moe):
        c, A = self.c, self.A
        j = i // 2
        last = (i == DEPTH - 1)
        with c.stage():
            self.eps_const()
            Hst = c.sbuf("Hst", [128, KC, 768], F32)
            F = c.sbuf("F", [128, KC, 768], BF16)
            G = c.sbuf("G", [128, FC, 768], BF16)
            sH = c.dsem()
            sSt = c.dsem()
            tmp_rot = Rot(c, "ntmp", [128, 512], F32, 2)
            sq_rot = Rot(c, "nsq", [128, KC, 512], BF16, 1)
            rs_rot = Rot(c, "nrs", [128, 512], F32, 1)
            ps_n = Rot(c, "psn", [128, 512], F32, 1, "psum")
            ps1 = Rot(c, "ps1", [128, 512], F32, 2, "psum")
            ps3 = Rot(c, "ps3", [128, 512], F32, 2, "psum")
            ps2 = Rot(c, "ps2", [128, 512], F32, 2, "psum")
            w1r = Rot(c, "w1", [128, KC, 512], BF16, 2, dma=True)
            w3r = Rot(c, "w3", [128, KC, 512], BF16, 2, dma=True)
            w2r = Rot(c, "w2", [128, FC, 256], BF16, 2, dma=True)
            sr = Rot(c, "sil", [128, 512], F32, 3)
            if moe:
                F32t = c.sbuf("F32t", [128, KC, 768], F32)
                gateB = c.sbuf("gateB", [128, NE, 768], BF16)
                rt = c.sbuf("rt", [128, KC, NE], F32)
                brt = c.buf()
                srt = c.dsem()
                c.dma("sp", srt, [(rt[:], A["moe_router"][j].rearrange("(k p) e -> p k e", p=128))],
                      [self.bIn], [brt])
                psr = Rot(c, "psr", [128, 512], F32, 1, "psum")
                Lg = c.sbuf("Lg", [128, 6, 8], F32)
                S8 = c.sbuf("S8", [128, 6, 8], F32)
                Gt = c.sbuf("Gt", [128, 6, 8], F32)
                Et = c.sbuf("Et", [128, 6, 8], F32)
                Wg = c.sbuf("Wg", [128, 6, 2], F32)
                bLg, bS8, bGt, bEt, bWg = c.buf(), c.buf(), c.buf(), c.buf(), c.buf()
                dgm = Rot(c, "dgm", [128, 128], BF16, 2)
                sgr = Rot(c, "sgr", [128, 512], BF16, 3)
            items = []
            for st, tiles in enumerate(ST3):
                if last:
                    tiles = [t for t in tiles if t[0] < NLAT]
                items.append(("pre", st, tiles, None, None))
                for ex in range(NE if moe else 1):
                    for jb in range(6):
                        items.append(("p1", st, tiles, ex, jb))
                    for mp in range(4):
                        items.append(("p2", st, tiles, ex, mp))
                items.append(("post", st, tiles, None, None))
            loaded = {}
            S = {}
            PB = dict(bHt=[[c.buf() for _ in range(2)] for _ in range(KC)], bF=[c.buf() for _ in range(2)],
                      bG=[[c.buf() for _ in range(2)] for _ in range(FC)], bGB=[c.buf() for _ in range(2)])

            def weights(ex):
                if moe:
                    return A["moe_w1"][j, ex], A["moe_w3"][j, ex], A["moe_w2"][j, ex]
                return A["ffn_w1"][j], A["ffn_w3"][j], A["ffn_w2"][j]

            def load(idx):
                kind, st, tiles, ex, q = items[idx]
                if idx in loaded or kind in ("pre", "post"):
                    return
                W1, W3, W2 = weights(ex)
                if kind == "p1":
                    nf = 4 if q < 5 else 2
                    w1t, bw1, sw1 = w1r.next()
                    w3t, bw3, sw3 = w3r.next()
                    cs = slice(q * 512, q * 512 + nf * 128)
                    c.dma("pool", sw1, [(w1t[:, :, 0:nf * 128], W1[:, cs].rearrange("(k p) f -> p k f", p=128))],
                          [self.bIn], [bw1])
                    c.dma("pool", sw3, [(w3t[:, :, 0:nf * 128], W3[:, cs].rearrange("(k p) f -> p k f", p=128))],
                          [self.bIn], [bw3])
                    loaded[idx] = (w1t, bw1, w3t, bw3)
                else:
                    w2t, bw2, sw2 = w2r.next()
                    c.dma("pool", sw2, [(w2t[:], W2[:, q * 256:(q + 1) * 256].rearrange("(f p) c -> p f c", p=128))],
                          [self.bIn], [bw2])
                    loaded[idx] = (w2t, bw2)

            def pre(st, tiles):
                n0s = tiles[0][0]
                wst = sum(t[1] for t in tiles)
                bHt, bF, bG, bGB = PB["bHt"], PB["bF"], PB["bG"], PB["bGB"]
                allH = [b for row in bHt for b in row]
                S.update(n0s=n0s, wst=wst, bHt=bHt, allH=allH, bF=bF, bG=bG, bGB=bGB)
                fence = allH + bF + [b for row in bG for b in row] + bGB
                c.dma("sp", sH, [(Hst[:, :, 0:wst], A["Hd"][:, :, n0s:n0s + wst].rearrange("k p t -> p k t"))],
                      [self.bIn], fence)
                for nt, (n0, w) in enumerate(tiles):
                    o = n0 - n0s
                    x = 0 if n0 < NLAT else 1
                    self.norm_tile(lambda k: Hst[:, k, o:o + w], [bHt[k][nt] for k in range(KC)], w,
                                   lambda k: self.nmT[:, i, 1, k, x:x + 1], lambda k: self.modT[:, i, 3, k, x:x + 1],
                                   lambda k: F[:, k, o:o + w], [bF[nt]], tmp_rot, sq_rot, ps_n, rs_rot,
                                   out32=(lambda k: F32t[:, k, o:o + w]) if moe else None)
                if moe:
                    blocks = [(nt, n0 - n0s + tb * 128) for nt, (n0, w) in enumerate(tiles) for tb in range(w // 128)]
                    nb6 = len(blocks)
                    ps, bps, _ = psr.next()
                    for bi, (nt, off) in enumerate(blocks):
                        for k in range(KC):
                            mm(c, ps[:, bi * 8:(bi + 1) * 8], F32t[:, k, off:off + 128], rt[:, k, :],
                               k == 0, k == KC - 1, [bF[nt], brt], [bps])
                    L3 = Lg[:, 0:nb6, :]
                    c.op("dve", lambda e: e.tensor_copy(out=L3, in_=ps[:, 0:nb6 * 8].rearrange("p (a b) -> p a b", b=8)),
                         [bps], [bLg])
                    for bi in range(nb6):
                        c.op("dve", lambda e: e.max(out=S8[:, bi, :], in_=Lg[:, bi, :]), [bLg], [bS8])
                    c.op("dve", lambda e: e.tensor_tensor(out=Wg[:, 0:nb6, 0], in0=S8[:, 0:nb6, 0], in1=S8[:, 0:nb6, 1],
                                                          op=ALU.subtract), [bS8], [bWg])
                    act(c, Wg[:, 0:nb6, 0], Wg[:, 0:nb6, 0], AF.Sigmoid, [bWg], [bWg])
                    c.op("dve", lambda e: e.tensor_scalar(out=Wg[:, 0:nb6, 1], in0=Wg[:, 0:nb6, 0], scalar1=-1.0,
                                                          scalar2=1.0, op0=ALU.mult, op1=ALU.add), [bWg], [bWg])
                    shp = [128, nb6, 8]
                    G3, E3 = Gt[:, 0:nb6, :], Et[:, 0:nb6, :]
                    c.op("dve", lambda e: e.tensor_tensor(out=G3, in0=L3, in1=S8[:, 0:nb6, 0:1].broadcast_to(shp),
                                                          op=ALU.is_equal), [bLg, bS8], [bGt])
                    c.op("dve", lambda e: e.tensor_tensor(out=G3, in0=G3, in1=Wg[:, 0:nb6, 0:1].broadcast_to(shp),
                                                          op=ALU.mult), [bGt, bWg], [bGt])
                    c.op("dve", lambda e: e.tensor_tensor(out=E3, in0=L3, in1=S8[:, 0:nb6, 1:2].broadcast_to(shp),
                                                          op=ALU.is_equal), [bLg, bS8], [bEt])
                    c.op("dve", lambda e: e.tensor_tensor(out=E3, in0=E3, in1=Wg[:, 0:nb6, 1:2].broadcast_to(shp),
                                                          op=ALU.mult), [bEt, bWg], [bEt])
                    c.op("dve", lambda e: e.tensor_tensor(out=G3, in0=G3, in1=E3, op=ALU.add), [bGt, bEt], [bGt])
                    for bi, (nt, off) in enumerate(blocks):
                        for ex in range(NE):
                            dm, bdm, _ = dgm.next()
                            c.op("dve", lambda e: e.tensor_scalar(out=dm[:], in0=self.identb[:],
                                                                  scalar1=Gt[:, bi, ex:ex + 1], scalar2=None,
                                                                  op0=ALU.mult), [bGt, self.bP], [bdm])
                            ps_, bps_, _ = psr.next()
                            mm(c, ps_[:, 0:128], self.onesb[:], dm[:], True, True, [bdm, self.bP], [bps_])
                            c.op("act", lambda e: e.copy(out=gateB[:, ex, off:off + 128], in_=ps_[:, 0:128]),
                                 [bps_], [bGB[nt]])

            def phase1(idx, tiles, ex, jb):
                w1t, bw1, w3t, bw3 = loaded.pop(idx)
                n0s, bF, bG, bGB = S["n0s"], S["bF"], S["bG"], S["bGB"]
                nf = 4 if jb < 5 else 2
                for q in range(nf):
                    f = jb * 4 + q
                    for nt, (n0, w) in enumerate(tiles):
                        o = n0 - n0s
                        p1, bp1, _ = ps1.next()
                        p3, bp3, _ = ps3.next()
                        for k in range(KC):
                            mm(c, p1[:, 0:w], w1t[:, k, q * 128:(q + 1) * 128], F[:, k, o:o + w],
                               k == 0, k == KC - 1, [bw1, bF[nt]], [bp1])
                        for k in range(KC):
                            mm(c, p3[:, 0:w], w3t[:, k, q * 128:(q + 1) * 128], F[:, k, o:o + w],
                               k == 0, k == KC - 1, [bw3, bF[nt]], [bp3])
                        s_, bs_, _ = sr.next()
                        act(c, s_[:, 0:w], p1[:, 0:w], AF.Silu, [bp1], [bs_])
                        if moe:
                            sg_, bsg_, _ = sgr.next()
                            c.op("dve", lambda e: e.tensor_tensor(out=sg_[:, 0:w], in0=s_[:, 0:w],
                                                                  in1=p3[:, 0:w], op=ALU.mult),
                                 [bs_, bp3], [bsg_])
                            c.op("pool", lambda e: e.tensor_tensor(out=G[:, f, o:o + w], in0=sg_[:, 0:w],
                                                                   in1=gateB[:, ex, o:o + w], op=ALU.mult),
                                 [bsg_, bGB[nt]], [bG[f][nt]])
                        else:
                            c.op("dve", lambda e: e.tensor_tensor(out=G[:, f, o:o + w], in0=s_[:, 0:w],
                                                                  in1=p3[:, 0:w], op=ALU.mult),
                                 [bs_, bp3], [bG[f][nt]])

            def phase2(idx, tiles, ex, mp):
                w2t, bw2 = loaded.pop(idx)
                n0s, bG, bHt = S["n0s"], S["bG"], S["bHt"]
                for mm_ in range(2):
                    m = mp * 2 + mm_
                    for nt, (n0, w) in enumerate(tiles):
                        o = n0 - n0s
                        x = 0 if n0 < NLAT else 1
                        p2, bp2, _ = ps2.next()
                        for f in range(FC):
                            mm(c, p2[:, 0:w], w2t[:, f, mm_ * 128:(mm_ + 1) * 128], G[:, f, o:o + w],
                               f == 0, f == FC - 1, [bw2, bG[f][nt]], [bp2])
                        c.op("dve", lambda e: e.scalar_tensor_tensor(
                            out=Hst[:, m, o:o + w], in0=p2[:, 0:w], scalar=self.modT[:, i, 5, m, x:x + 1],
                            in1=Hst[:, m, o:o + w], op0=ALU.mult, op1=ALU.add),
                            [bp2, bHt[m][nt], self.bP], [bHt[m][nt]])

            def post():
                n0s, wst = S["n0s"], S["wst"]
                c.dma("sp", sSt, [(A["Hd"][:, :, n0s:n0s + wst].rearrange("k p t -> p k t"), Hst[:, :, 0:wst])],
                      S["allH"], [c.buf()])

            widx = [q for q, it in enumerate(items) if it[0] in ("p1", "p2")]
            ada = self.adaln_pieces(i + 1, ps_n) if (i + 1 < DEPTH and not self.stages) else []
            every = max(1, (len(items) - 4) // (len(ada) + 1)) if ada else 0
            for idx, (kind, st, tiles, ex, q) in enumerate(items):
                if ada and kind in ("p1", "p2") and idx % every == 0:
                    ada.pop(0)()
                nxt = [w_ for w_ in widx if w_ >= idx][:2]
                if kind in ("p1", "p2"):
                    for w_ in nxt:
                        load(w_)
                else:
                    for w_ in nxt[:1]:
                        load(w_)
                if kind == "pre":
                    pre(st, tiles)
                elif kind == "p1":
                    phase1(idx, tiles, ex, q)
                elif kind == "p2":
                    phase2(idx, tiles, ex, q)
                else:
                    post()
            while ada:
                ada.pop(0)()

    def stage_final(self):
        c, A = self.c, self.A
        with c.stage():
            self.eps_const()
            hr = Rot(c, "fh", [128, KC, 512], F32, 2, dma=True)
            o32 = Rot(c, "fo", [128, KC, 512], F32, 2)
            tmp_rot = Rot(c, "ntmp", [128, 512], F32, 2)
            sq_rot = Rot(c, "nsq", [128, KC, 512], BF16, 1)
            rs_rot = Rot(c, "nrs", [128, 512], F32, 1)
            ps_n = Rot(c, "psn", [128, 512], F32, 1, "psum")
            pr = Rot(c, "pout", [128, 512], F32, 4, "psum")
            orow = Rot(c, "orow", [128, D], F32, 3, dma=True)
            for nt, (n0, w) in enumerate(NT5[:4]):
                ht, bh, sh = hr.next()
                c.dma("sp", sh, [(ht[:, :, 0:w], A["Hd"][:, :, n0:n0 + w].rearrange("k p t -> p k t"))],
                      [self.bIn], [bh])
                ot, bo, _ = o32.next()
                self.norm_tile(lambda k: ht[:, k, 0:w], [bh], w, lambda k: self.vcol(R_FING, k), None,
                               lambda k: ot[:, k, 0:w], [bo], tmp_rot, sq_rot, ps_n, rs_rot)
                for tb in range(w // 128):
                    rw, brw, srw = orow.next()
                    for g in range(2):
                        ps, bps, _ = pr.next()
                        for q in range(4):
                            k = g * 4 + q
                            c.op("pe", lambda e: e.transpose(out=ps[:, q * 128:(q + 1) * 128],
                                                             in_=ot[:, k, tb * 128:(tb + 1) * 128],
                                                             identity=self.identf[:]), [bo, self.bP], [bps])
                        if g == 0:
                            c.op("dve", lambda e: e.tensor_copy(out=rw[:, 0:512], in_=ps[:]), [bps], [brw])
                        else:
                            c.op("act", lambda e: e.copy(out=rw[:, 512:1024], in_=ps[:]), [bps], [brw])
                    c.dma("sp", srw, [(A["out"][n0 + tb * 128:n0 + (tb + 1) * 128, :], rw[:])], [brw], [self.bOut])

    def stage_dump(self, ctx=False):
        c, A = self.c, self.A
        tl = [(2048, 256, 0)] if ctx else [(n0, w, n0) for (n0, w) in NT5[:4]]
        with c.stage():
            hr = Rot(c, "fh", [128, KC, 512], F32, 2, dma=True)
            pr = Rot(c, "pout", [128, 512], F32, 4, "psum")
            orow = Rot(c, "orow", [128, D], F32, 3, dma=True)
            for nt, (n0, w, o0) in enumerate(tl):
                ht, bh, sh = hr.next()
                c.dma("sp", sh, [(ht[:, :, 0:w], A["Hd"][:, :, n0:n0 + w].rearrange("k p t -> p k t"))],
                      [self.bIn], [bh])
                for tb in range(w // 128):
                    rw, brw, srw = orow.next()
                    for g in range(2):
                        ps, bps, _ = pr.next()
                        for q in range(4):
                            k = g * 4 + q
                            c.op("pe", lambda e: e.transpose(out=ps[:, q * 128:(q + 1) * 128],
                                                             in_=ht[:, k, tb * 128:(tb + 1) * 128],
                                                             identity=self.identf[:]), [bh, self.bP], [bps])
                        if g == 0:
                            c.op("dve", lambda e: e.tensor_copy(out=rw[:, 0:512], in_=ps[:]), [bps], [brw])
                        else:
                            c.op("act", lambda e: e.copy(out=rw[:, 512:1024], in_=ps[:]), [bps], [brw])
                    c.dma("sp", srw, [(A["out"][o0 + tb * 128:o0 + (tb + 1) * 128, :], rw[:])], [brw], [self.bOut])

    def stage_conv(self, i):
        c, A = self.c, self.A
        j = i // 2
        with c.stage():
            self.eps_const()
            Aact = c.sbuf("A", [128, KC, NTOK], BF16)
            V = c.sbuf("V", [128, KC, NTOK], BF16)
            bA = [c.buf() for _ in NT5]
            bV = [[c.buf() for _ in NT5] for _ in range(KC)]
            bS = [c.buf() for _ in NT5]
            with c.stage():
                hr = Rot(c, "ch", [128, KC, 512], F32, 2, dma=True)
                tmp_rot = Rot(c, "ntmp", [128, 512], F32, 2)
                sq_rot = Rot(c, "nsq", [128, KC, 512], BF16, 1)
                rs_rot = Rot(c, "nrs", [128, 512], F32, 1)
                ps_n = Rot(c, "psn", [128, 512], F32, 2, "psum")
                for nt, (n0, w) in enumerate(NT5):
                    x = 0 if n0 < NLAT else 1
                    ht, bh, sh = hr.next()
                    c.dma("sp", sh, [(ht[:, :, 0:w], A["Hd"][:, :, n0:n0 + w].rearrange("k p t -> p k t"))],
                          [self.bIn], [bh])
                    self.norm_tile(lambda k: ht[:, k, 0:w], [bh], w,
                                   lambda k: self.nmT[:, i, 0, k, x:x + 1], lambda k: self.modT[:, i, 0, k, x:x + 1],
                                   lambda k: Aact[:, k, n0:n0 + w], [bA[nt]], tmp_rot, sq_rot, ps_n, rs_rot)
            with c.stage():
                LW, LH = 32 * 94, 62 * 64
                upW = Rot(c, "upW", [128, LW + 286], BF16, 2)
                upH = Rot(c, "upH", [128, LH + 286], BF16, 2)
                for r_ in (upW, upH):
                    for q in range(2):
                        c.op("pool", lambda e: e.memset(r_.t[q][:], 0.0), [], [r_.b[q]])
                dgr = Rot(c, "dg", [128, CW, 128], BF16, 2)
                w1r = Rot(c, "cw1", [128, 2, KC, 128], BF16, 2, dma=True)
                sgr = Rot(c, "sg", [128, 512], F32, 2)
                psl = Rot(c, "psl", [128, 512], F32, 2, "psum")
                psg = Rot(c, "psg", [128, 512], F32, 2, "psum")
                psc = Rot(c, "psc", [128, 512], F32, 3, "psum")
                W1 = A["conv_pw1_w"][j]
                for ch in range(KC):
                    wtype = ch < 4
                    up, bup, _ = (upW if wtype else upH).next()
                    LB = LW if wtype else LH
                    w1t, bw1, sw1 = w1r.next()
                    c.dma("pool", sw1,
                          [(w1t[:, 0], W1[:, ch * 128:(ch + 1) * 128].rearrange("(k p) c -> p k c", p=128)),
                           (w1t[:, 1], W1[:, D + ch * 128:D + (ch + 1) * 128].rearrange("(k p) c -> p k c", p=128))],
                          [self.bIn], [bw1])
                    dg, bdg, _ = dgr.next()
                    for k in range(CW):
                        c.op("dve", lambda e: e.tensor_scalar(out=dg[:, k, :], in0=self.identb[:],
                                                              scalar1=self.vcol(R_DWW + j * CW + k, ch), scalar2=None,
                                                              op0=ALU.mult), [self.bP], [bdg])

                    def upview(n0, w, k):
                        if n0 >= NLAT:
                            return up[:, LB + k:LB + k + 256], False
                        r0 = n0 // 64
                        if wtype:
                            v = up[:, r0 * 94:(r0 + 8) * 94].rearrange("p (r q) -> p r q", q=94)
                            return v[:, :, k:k + 64], True
                        return up[:, (r0 + k) * 64:(r0 + k) * 64 + 512], False

                    for nt, (n0, w) in enumerate(NT5):
                        pl, bpl, _ = psl.next()
                        pg, bpg, _ = psg.next()
                        for k in range(KC):
                            mm(c, pl[:, 0:w], w1t[:, 0, k, :], Aact[:, k, n0:n0 + w], k == 0, k == KC - 1,
                               [bw1, bA[nt]], [bpl])
                        for k in range(KC):
                            mm(c, pg[:, 0:w], w1t[:, 1, k, :], Aact[:, k, n0:n0 + w], k == 0, k == KC - 1,
                               [bw1, bA[nt]], [bpg])
                        sg, bsg, _ = sgr.next()
                        act(c, sg[:, 0:w], pg[:, 0:w], AF.Sigmoid, [bpg, self.bP], [bsg],
                            bias=self.vcol(R_PW1B + j * 2 + 1, ch))
                        ov, is3 = upview(n0, w, 15)
                        if is3:
                            i0 = pl[:, 0:w].rearrange("p (r q) -> p r q", q=64)
                            i1 = sg[:, 0:w].rearrange("p (r q) -> p r q", q=64)
                        else:
                            i0, i1 = pl[:, 0:w], sg[:, 0:w]
                        c.op("dve", lambda e: e.scalar_tensor_tensor(
                            out=ov, in0=i0, scalar=self.vcol(R_PW1B + j * 2, ch), in1=i1,
                            op0=ALU.add, op1=ALU.mult), [bpl, bsg, self.bP], [bup])
                    for nt, (n0, w) in enumerate(NT5):
                        pc, bpc, _ = psc.next()
                        for k in range(CW):
                            rv, is3 = upview(n0, w, k)
                            ov = pc[:, 0:w].rearrange("p (r q) -> p r q", q=64) if is3 else pc[:, 0:w]
                            mm(c, ov, dg[:, k, :], rv, k == 0, k == CW - 1, [bdg, bup], [bpc])
                        act(c, V[:, ch, n0:n0 + w], pc[:, 0:w], AF.Identity, [bpc, self.bP], [bV[ch][nt]],
                            bias=self.vcol(R_DWB + j, ch))
            with c.stage():
                sq_rot = Rot(c, "lsq", [128, KC, 512], BF16, 2)
                ps_s = Rot(c, "pss", [128, 512], F32, 2, "psum")
                ps_q = Rot(c, "psq", [128, 512], F32, 2, "psum")
                mr = Rot(c, "lmean", [128, 512], F32, 2)
                vr = Rot(c, "lvar", [128, 512], F32, 2)
                t1r = Rot(c, "lt1", [128, 512], F32, 3)
                for nt, (n0, w) in enumerate(NT5):
                    sq, bsq, _ = sq_rot.next()
                    allV = [bV[k][nt] for k in range(KC)]
                    for k in range(KC):
                        act(c, sq[:, k, 0:w], V[:, k, n0:n0 + w], AF.Square, [bV[k][nt]], [bsq])
                    pS, bpS, _ = ps_s.next()
                    pQ, bpQ, _ = ps_q.next()
                    for k in range(KC):
                        mm(c, pS[:, 0:w], self.onesb[:], V[:, k, n0:n0 + w], k == 0, k == KC - 1,
                           [bV[k][nt], self.bP], [bpS])
                    for k in range(KC):
                        mm(c, pQ[:, 0:w], self.onesb[:], sq[:, k, 0:w], k == 0, k == KC - 1, [bsq, self.bP], [bpQ])
                    mean, bm, _ = mr.next()
                    var, bv, _ = vr.next()
                    act(c, mean[:, 0:w], pS[:, 0:w], AF.Identity, [bpS], [bm], scale=1.0 / D)
                    c.op("dve", lambda e: e.tensor_tensor(out=var[:, 0:w], in0=mean[:, 0:w], in1=mean[:, 0:w],
                                                          op=ALU.mult), [bm], [bv])
                    c.op("dve", lambda e: e.scalar_tensor_tensor(out=var[:, 0:w], in0=pQ[:, 0:w], scalar=1.0 / D,
                                                                 in1=var[:, 0:w], op0=ALU.mult, op1=ALU.subtract),
                         [bpQ, bv], [bv])
                    act(c, var[:, 0:w], var[:, 0:w], AF.Ln, [bv], [bv], bias=self.epsc[:, 0:1])
                    act(c, var[:, 0:w], var[:, 0:w], AF.Exp, [bv], [bv], scale=-0.5)
                    for k in range(KC):
                        t1, bt1, _ = t1r.next()
                        c.op("dve", lambda e: e.tensor_tensor(out=t1[:, 0:w], in0=V[:, k, n0:n0 + w],
                                                              in1=mean[:, 0:w], op=ALU.subtract),
                             [bV[k][nt], bm], [bt1])
                        c.op("dve", lambda e: e.tensor_tensor(out=t1[:, 0:w], in0=t1[:, 0:w], in1=var[:, 0:w],
                                                              op=ALU.mult), [bt1, bv], [bt1])
                        act(c, Aact[:, k, n0:n0 + w], t1[:, 0:w], AF.Silu, [bt1, self.bP], [bS[nt]],
                            scale=self.vcol(R_LNG + j, k), bias=self.vcol(R_LNB + j, k))
            with c.stage():
                w2 = c.sbuf("cw2", [128, KC, D], BF16)
                bw2 = c.buf()
                sw2 = c.dsem()
                c.dma("pool", sw2, [(w2[:], A["conv_pw2_w"][j].rearrange("(k p) m -> p k m", p=128))],
                      [self.bIn], [bw2])
                self.residual_out(i, 2, w2, bw2, Aact, bS, NT5, bias_row=R_PW2B + j)

    def residual_out(self, i, gpart, w2, bw2, S, bS, tiles, bias_row=None):
        c, A = self.c, self.A
        bgc = c.sbuf("bgc", [128, KC, 2], F32)
        bbg = c.buf()
        if bias_row is not None:
            for x in range(2):
                c.op("dve", lambda e: e.tensor_tensor(out=bgc[:, :, x], in0=self.vT[:, :, bias_row],
                                                      in1=self.modT[:, i, gpart, :, x], op=ALU.mult),
                     [self.bP], [bbg])
        hr = Rot(c, "rh", [128, 512], F32, 4, dma=True)
        hs = [c.dsem() for _ in range(4)]
        tr = Rot(c, "rtmp", [128, 512], F32, 2)
        ps = Rot(c, "rps", [128, 512], F32, 3, "psum")
        items = [(nt, n0, w, m) for nt, (n0, w) in enumerate(tiles) for m in range(KC)]
        slots = {}

        def load(idx):
            nt, n0, w, m = items[idx]
            ht, bh, sh = hr.next()
            slots[idx] = (ht, bh, hs[idx % 4])
            c.dma("sp", sh, [(ht[:, 0:w], A["Hd"][m, :, n0:n0 + w])], [self.bIn], [bh])

        for idx in range(min(2, len(items))):
            load(idx)
        for idx, (nt, n0, w, m) in enumerate(items):
            if idx + 2 < len(items):
                load(idx + 2)
            x = 0 if n0 < NLAT else 1
            ht, bh, sst = slots.pop(idx)
            p, bp, _ = ps.next()
            for k in range(KC):
                mm(c, p[:, 0:w], w2[:, k, m * 128:(m + 1) * 128], S[:, k, n0:n0 + w], k == 0, k == KC - 1,
                   [bw2, bS[nt]], [bp])
            tmp, btmp, _ = tr.next()
            if bias_row is not None:
                act(c, tmp[:, 0:w], p[:, 0:w], AF.Identity, [bp, self.bP, bbg], [btmp],
                    scale=self.modT[:, i, gpart, m, x:x + 1], bias=bgc[:, m, x:x + 1])
            else:
                act(c, tmp[:, 0:w], p[:, 0:w], AF.Identity, [bp, self.bP], [btmp],
                    scale=self.modT[:, i, gpart, m, x:x + 1])
            c.op("dve", lambda e: e.tensor_tensor(out=ht[:, 0:w], in0=ht[:, 0:w], in1=tmp[:, 0:w], op=ALU.add),
                 [bh, btmp], [bh])
            c.dma("sp", sst, [(A["Hd"][m, :, n0:n0 + w], ht[:, 0:w])], [bh], [bh])

    def stage_hgrn(self, i):
        c, A = self.c, self.A
        j = i // 2
        which = 0 if i == 1 else 1
        last = (i == DEPTH - 1)
        NB = NTOK // 128
        NCH = NTOK // GC
        LNQ = float(-0.5 * np.log(128.0))
        with c.stage():
            self.eps_const()
            Aact = c.sbuf("A", [128, KC, NTOK], BF16)
            bA = [c.buf() for _ in NT5]
            rstm = c.sbuf("rstm", [128, NTOK], F32)
            mF = c.sbuf("mF", [128, 2, 128], F32)
            lnq = c.sbuf("lnq", [128, 1], F32)
            bC = c.buf()
            sC = c.dsem()
            c.dma("sp", sC, [(rstm[:], A["consts"][:, C_RST:C_END]),
                             (mF[:, 0, :], A["consts"][:, C_MF:C_MF + 128]),
                             (mF[:, 1, :], A["consts"][:, C_MB:C_MB + 128])], [self.bIn], [bC])
            c.op("dve", lambda e: e.memset(lnq[:], LNQ), [], [bC])
            with c.stage():
                hr = Rot(c, "ch", [128, KC, 512], F32, 2, dma=True)
                tmp_rot = Rot(c, "ntmp", [128, 512], F32, 2)
                sq_rot = Rot(c, "nsq", [128, KC, 512], BF16, 1)
                rs_rot = Rot(c, "nrs", [128, 512], F32, 1)
                ps_n = Rot(c, "psn", [128, 512], F32, 2, "psum")
                for nt, (n0, w) in enumerate(NT5):
                    x = 0 if n0 < NLAT else 1
                    ht, bh, sh = hr.next()
                    c.dma("sp", sh, [(ht[:, :, 0:w], A["Hd"][:, :, n0:n0 + w].rearrange("k p t -> p k t"))],
                          [self.bIn], [bh])
                    self.norm_tile(lambda k: ht[:, k, 0:w], [bh], w,
                                   lambda k: self.nmT[:, i, 0, k, x:x + 1], lambda k: self.modT[:, i, 0, k, x:x + 1],
                                   lambda k: Aact[:, k, n0:n0 + w], [bA[nt]], tmp_rot, sq_rot, ps_n, rs_rot)
            with c.stage():
                whr = Rot(c, "wh", [128, 5, KC, 128], BF16, 2, dma=True)
                lf = [c.sbuf("lf", [128, NTOK], F32) for _ in range(2)]
                kk = [c.sbuf("kk", [128, NTOK], BF16) for _ in range(2)]
                qs = c.sbuf("qs", [128, NTOK], BF16)
                gs2 = [c.sbuf("gs", [128, NTOK], BF16) for _ in range(2)]
                vtok2 = [c.sbuf("vtok", [128, NB, 128], BF16) for _ in range(2)]
                Rh = Rot(c, "Rh", [128, NTOK], BF16, 2, dma=True)
                bc = c.sbuf("bc", [128, NTOK], F32)
                qin = [c.sbuf("qin", [128, NTOK], BF16) for _ in range(2)]
                kin = [c.sbuf("kin", [128, NTOK], BF16) for _ in range(2)]
                kstT = c.sbuf("kstT", [128, NTOK], BF16)
                ksttok = [c.sbuf("ksttok", [128, NB, 128], BF16) for _ in range(2)]
                kst3 = [c.sbuf("kst3", [128, NB, 128], BF16) for _ in range(2)]
                decs = [c.sbuf("decs", [128, NCH], F32) for _ in range(2)]
                oT = c.sbuf("oT", [128, NTOK], F32)
                s32r = [Rot(c, "S32", [128, 128], F32, 3) for _ in range(2)]
                sbr = [Rot(c, "Sb", [128, 128], BF16, 3) for _ in range(2)]
                tr = Rot(c, "etmp", [128, 512], F32, 3)
                amr = [Rot(c, "attm", [128, 128], BF16, 2) for _ in range(2)]
                sqr = Rot(c, "osq", [128, 512], BF16, 1)
                rsr = Rot(c, "ors", [128, 512], F32, 1)
                pg = Rot(c, "pg", [128, 512], F32, 2, "psum")
                pT = Rot(c, "pT", [128, 1024], BF16, 1, "psum")
                pa = Rot(c, "pa", [128, 512], F32, 1, "psum")
                po = Rot(c, "po", [128, 512], F32, 2, "psum")
                pk = Rot(c, "pk", [128, 512], F32, 2, "psum")
                Win = A["rec_w_in"][j]
                blf = [[c.buf() for _ in NT5] for _ in range(2)]
                bkk = [[c.buf() for _ in NT5] for _ in range(2)]
                bqs = [c.buf() for _ in NT5]
                bgs2 = [[c.buf() for _ in NT5] for _ in range(2)]
                bvt2 = [c.buf(), c.buf()]
                bo = [c.buf() for _ in range(NB)]
                bbc = c.buf()
                bqi = [[c.buf() for _ in NT5] for _ in range(2)]
                bki = [[c.buf() for _ in NT5] for _ in range(2)]
                bks = [c.buf() for _ in NT5]
                bdec = [c.buf(), c.buf()]
                bkt = [c.buf(), c.buf()]
                pgp, bpgp = pg.t[1], pg.b[1]

                def proj_thunks(h):
                    hb = h % 2
                    st = {}
                    th = []

                    def t_load():
                        wh, bwh, swh = whr.next()
                        st.update(wh=wh, bwh=bwh)
                        c.dma("pool", swh, [(wh[:, part], Win[:, part * D + h * 128:part * D + (h + 1) * 128]
                                             .rearrange("(k p) c -> p k c", p=128)) for part in range(5)],
                              [self.bIn], [bwh])
                    th.append(t_load)

                    def vgroup(g):
                        def run():
                            wh, bwh = st["wh"], st["bwh"]
                            nb_ = min(4, NB - g * 4)
                            for q in range(nb_):
                                blk = g * 4 + q
                                for k in range(KC):
                                    mm(c, pgp[:, q * 128:(q + 1) * 128], Aact[:, k, blk * 128:(blk + 1) * 128],
                                       wh[:, 0, k, :], k == 0, k == KC - 1, [bwh] + bA, [bpgp])
                            c.op("act", lambda e: e.copy(out=vtok2[hb][:, g * 4:g * 4 + nb_, :],
                                                         in_=pgp[:, 0:nb_ * 128].rearrange("p (a b) -> p a b", b=128)),
                                 [bpgp], [bvt2[hb]])
                        return run
                    for g in range((NB + 3) // 4):
                        th.append(vgroup(g))

                    def zgroup(part, nt):
                        def run():
                            wh, bwh = st["wh"], st["bwh"]
                            n0, w = NT5[nt]
                            for k in range(KC):
                                mm(c, pgp[:, 0:w], wh[:, part, k, :], Aact[:, k, n0:n0 + w], k == 0, k == KC - 1,
                                   [bwh, bA[nt]], [bpgp])
                            if part in (1, 2):
                                act(c, lf[part - 1][:, n0:n0 + w], pgp[:, 0:w], AF.Sigmoid, [bpgp],
                                    [blf[part - 1][nt]])
                            elif part == 3:
                                act(c, qs[:, n0:n0 + w], pgp[:, 0:w], AF.Silu, [bpgp], [bqs[nt]])
                            else:
                                act(c, gs2[hb][:, n0:n0 + w], pgp[:, 0:w], AF.Silu, [bpgp], [bgs2[hb][nt]])
                        return run

                    def fk():
                        for d in range(2):
                            c.op("dve", lambda e: e.tensor_scalar(
                                out=lf[d][:], in0=lf[d][:], scalar1=self.lbT[:, h, d, which, 1:2],
                                scalar2=self.lbT[:, h, d, which, 0:1], op0=ALU.mult, op1=ALU.add),
                                blf[d] + [self.bP], blf[d])
                            c.op("dve", lambda e: e.tensor_scalar(
                                out=kk[d][:], in0=lf[d][:], scalar1=-1.0, scalar2=1.0,
                                op0=ALU.mult, op1=ALU.add), blf[d], bkk[d])
                        for d in range(2):
                            act(c, lf[d][:], lf[d][:], AF.Ln, blf[d], blf[d])
                    for part in (1, 2, 3, 4):
                        for nt in range(len(NT5)):
                            th.append(zgroup(part, nt))
                        if part == 2:
                            th.append(fk)
                    return th

                for th_ in proj_thunks(0):
                    th_()
                kvbanks = [(pk.t[0], pk.b[0]), (pk.t[1], pk.b[1]), (pg.t[0], pg.b[0]),
                           (pT.t[0][:].bitcast(F32), pT.b[0])]
                for h in range(KC):
                    hb = h % 2
                    nxt = proj_thunks(h + 1) if h + 1 < KC else []
                    for d in range(2):
                        c.op("dve", lambda e: e.tensor_tensor_scan(out=bc[:], data0=rstm[:], data1=lf[d][:],
                                                                   initial=0.0, op0=ALU.mult, op1=ALU.add),
                             blf[d] + [bC], [bbc])
                        for nt, (n0, w) in enumerate(NT5):
                            nc_ = w // GC
                            bc3 = bc[:, n0:n0 + w].rearrange("p (a b) -> p a b", b=GC)
                            if d == 1:
                                t0, bt0, _ = tr.next()
                                c.op("dve", lambda e: e.tensor_tensor(
                                    out=t0[:, 0:w].rearrange("p (a b) -> p a b", b=GC),
                                    in0=bc3[:, :, GC - 1:GC].broadcast_to([128, nc_, GC]), in1=bc3,
                                    op=ALU.subtract), [bbc], [bt0])
                                c.op("dve", lambda e: e.tensor_tensor(out=bc[:, n0:n0 + w], in0=t0[:, 0:w],
                                                                      in1=lf[d][:, n0:n0 + w], op=ALU.add),
                                     [bt0, blf[d][nt]], [bbc])
                            tot = bc3[:, :, GC - 1:GC] if d == 0 else bc3[:, :, 0:1]
                            t1, bt1, _ = tr.next()
                            act(c, t1[:, 0:w], bc[:, n0:n0 + w], AF.Exp, [bbc, bC], [bt1], bias=lnq[:, 0:1])
                            c.op("dve", lambda e: e.tensor_tensor(out=qin[d][:, n0:n0 + w], in0=qs[:, n0:n0 + w],
                                                                  in1=t1[:, 0:w], op=ALU.mult),
                                 [bt1, bqs[nt]], [bqi[d][nt]])
                            t2, bt2, _ = tr.next()
                            act(c, t2[:, 0:w], bc[:, n0:n0 + w], AF.Exp, [bbc], [bt2], scale=-1.0)
                            c.op("pool", lambda e: e.tensor_tensor(out=kin[d][:, n0:n0 + w],
                                                                   in0=kk[d][:, n0:n0 + w],
                                                                   in1=t2[:, 0:w], op=ALU.mult),
                                 [bt2, bkk[d][nt]], [bki[d][nt]])
                            t3, bt3, _ = tr.next()
                            c.op("dve", lambda e: e.tensor_tensor(
                                out=t3[:, 0:w].rearrange("p (a b) -> p a b", b=GC),
                                in0=tot.broadcast_to([128, nc_, GC]), in1=bc3, op=ALU.subtract), [bbc], [bt3])
                            act(c, t3[:, 0:w], t3[:, 0:w], AF.Exp, [bt3], [bt3])
                            c.op("pool" if nt % 2 == 0 else "dve",
                                 lambda e: e.tensor_tensor(out=kstT[:, n0:n0 + w], in0=kk[d][:, n0:n0 + w],
                                                           in1=t3[:, 0:w], op=ALU.mult),
                                 [bt3, bkk[d][nt]], [bks[nt]])
                            act(c, decs[d][:, n0 // GC:n0 // GC + nc_], tot, AF.Exp, [bbc], [bdec[d]])
                        for g in range((NB + 3) // 4):
                            p, bp, _ = pT.next()
                            nb_ = min(4, NB - g * 4)
                            for q in range(nb_):
                                blk = g * 4 + q
                                c.op("pe", lambda e: e.transpose(out=p[:, q * 128:(q + 1) * 128],
                                                                 in_=kstT[:, blk * 128:(blk + 1) * 128],
                                                                 identity=self.identb[:]), bks + [self.bP], [bp])
                            c.op("dve", lambda e: e.tensor_copy(
                                out=ksttok[d][:, g * 4:g * 4 + nb_, :],
                                in_=p[:, 0:nb_ * 128].rearrange("p (a b) -> p a b", b=128)), [bp], [bkt[d]])
                            c.op("dve", lambda e: e.tensor_scalar(
                                out=kst3[d][:, g * 4:g * 4 + nb_, :],
                                in0=p[:, 0:nb_ * 128].rearrange("p (a b) -> p a b", b=128),
                                scalar1=mF[:, 0, 127:128], scalar2=None, op0=ALU.mult), [bp, bC], [bkt[d]])
                    orders = [[16, 17] + list(range(16)), [17, 16] + list(range(15, -1, -1))]
                    chl = [[0, 1, 2, 3], [3, 2, 1, 0]]
                    chunks = [[(blk, cc) for blk in orders[d] for cc in chl[d]] for d in range(2)]
                    ST = []
                    for d in range(2):
                        S32, bS32, _ = s32r[d].next()
                        c.op("dve", lambda e: e.memset(S32[:], 0.0), [], [bS32])
                        sb, bsb, _ = sbr[d].next()
                        c.op("pool", lambda e: e.memset(sb[:], 0.0), [], [bsb])
                        ST.append(dict(S32=S32, bS32=bS32, sb=sb, bsb=bsb, kvi=0))

                    def att(d, blk):
                        cols = slice(blk * 128, (blk + 1) * 128)
                        nt = min(blk // 4, 4)
                        pA, bpA, _ = pa.next()
                        mm(c, pA[:, 0:128], kin[d][:, cols], qin[d][:, cols], True, True,
                           [bki[d][nt], bqi[d][nt]], [bpA])
                        am, bam, _ = amr[d].next()
                        c.op("dve", lambda e: e.tensor_tensor(out=am[:], in0=pA[:, 0:128], in1=mF[:, d, :],
                                                              op=ALU.mult), [bpA, bC], [bam])
                        return am, bam

                    def kv(d, blk, cc):
                        pK, bpK = kvbanks[2 * d + ST[d]["kvi"] % 2]
                        ST[d]["kvi"] += 1
                        if cc < 3:
                            mm(c, pK[:, 0:128], ksttok[d][cc * GC:(cc + 1) * GC, blk, :],
                               vtok2[hb][cc * GC:(cc + 1) * GC, blk, :], True, True, [bkt[d], bvt2[hb]], [bpK])
                        else:
                            mm(c, pK[:, 0:128], kst3[d][64:128, blk, :], vtok2[hb][64:128, blk, :], True, True,
                               [bkt[d], bvt2[hb]], [bpK])
                        return pK, bpK

                    for d in range(2):
                        ST[d]["att"] = att(d, orders[d][0])
                        ST[d]["kv"] = kv(d, *chunks[d][0])
                    owritten = set()
                    for idx in range(len(chunks[0])):
                        if nxt and idx >= 2:
                            nxt.pop(0)()
                        for d in range(2):
                            X = ST[d]
                            blk, cc = chunks[d][idx]
                            cols = slice(blk * 128, (blk + 1) * 128)
                            nt = min(blk // 4, 4)
                            ci = idx % 4
                            if ci == 0:
                                am, bam = X["att"]
                                oi = idx // 4
                                if oi + 1 < NB:
                                    X["att"] = att(d, orders[d][oi + 1])
                                pO, bpO = po.t[d], po.b[d]
                                mm(c, pO[:, 0:128], vtok2[hb][:, blk, :], am[:], True, False, [bvt2[hb], bam], [bpO])
                            pO, bpO = po.t[d], po.b[d]
                            pK, bpK = X["kv"]
                            if idx + 1 < len(chunks[d]):
                                X["kv"] = kv(d, *chunks[d][idx + 1])
                            ch = blk * 4 + cc
                            mm(c, pO[:, cc * GC:(cc + 1) * GC], X["sb"][:], qin[d][:, ch * GC:(ch + 1) * GC],
                               False, ci == 3, [X["bsb"], bqi[d][nt]], [bpO])
                            S32n, bS32n, _ = s32r[d].next()
                            S32o, bS32o = X["S32"], X["bS32"]
                            c.op("dve", lambda e: e.scalar_tensor_tensor(
                                out=S32n[:], in0=S32o[:], scalar=decs[d][:, ch:ch + 1], in1=pK[:, 0:128],
                                op0=ALU.mult, op1=ALU.add), [bS32o, bdec[d], bpK], [bS32n])
                            X["S32"], X["bS32"] = S32n, bS32n
                            sb, bsb, _ = sbr[d].next()
                            c.op("act", lambda e: e.copy(out=sb[:], in_=S32n[:]), [bS32n], [bsb])
                            X["sb"], X["bsb"] = sb, bsb
                            if ci == 3:
                                if blk not in owritten:
                                    owritten.add(blk)
                                    c.op("act", lambda e: e.copy(out=oT[:, cols], in_=pO[:, 0:128]), [bpO], [bo[blk]])
                                else:
                                    c.op("dve", lambda e: e.tensor_tensor(out=oT[:, cols], in0=oT[:, cols],
                                                                          in1=pO[:, 0:128], op=ALU.add),
                                         [bpO, bo[blk]], [bo[blk]])
                    while nxt:
                        nxt.pop(0)()
                    rh, brh, srh = Rh.next()
                    for nt, (n0, w) in enumerate(NT5):
                        bos = bo[n0 // 128:(n0 + w) // 128]
                        sq, bsq, _ = sqr.next()
                        c.op("pool", lambda e: e.tensor_tensor(out=sq[:, 0:w], in0=oT[:, n0:n0 + w],
                                                               in1=oT[:, n0:n0 + w], op=ALU.mult), bos, [bsq])
                        mm(c, pgp[:, 0:w], self.onesb[:], sq[:, 0:w], True, True, [bsq, self.bP], [bpgp])
                        rs, brs, _ = rsr.next()
                        act(c, rs[:, 0:w], pgp[:, 0:w], AF.Ln, [bpgp], [brs], scale=1.0 / 128, bias=self.epsc[:, 0:1])
                        act(c, rs[:, 0:w], rs[:, 0:w], AF.Exp, [brs], [brs], scale=-0.5)
                        c.op("dve", lambda e: e.tensor_tensor(out=rs[:, 0:w], in0=rs[:, 0:w], in1=oT[:, n0:n0 + w],
                                                              op=ALU.mult), [brs] + bos, [brs])
                        c.op("dve", lambda e: e.scalar_tensor_tensor(
                            out=rh[:, n0:n0 + w], in0=rs[:, 0:w], scalar=self.vcol(R_ONG + j, h),
                            in1=gs2[hb][:, n0:n0 + w], op0=ALU.mult, op1=ALU.mult),
                            [brs, bgs2[hb][nt], self.bP], [brh])
                    c.dma("sp", srh, [(A["Rd"][h], rh[:])], [brh], [brh])
            with c.stage():
                w2 = c.sbuf("wo", [128, KC, D], BF16)
                bw2 = c.buf()
                sw2 = c.dsem()
                c.dma("pool", sw2, [(w2[:], A["rec_w_o"][j].rearrange("(k p) m -> p k m", p=128))],
                      [self.bIn], [bw2])
                Rr = c.sbuf("R", [128, KC, NTOK], BF16)
                bR1 = c.buf()
                sR = c.dsem()
                c.dma("sp", sR, [(Rr[:], A["Rd"].rearrange("k p t -> p k t"))], [self.bIn], [bR1])
                self.residual_out(i, 2, w2, bw2, Rr, [bR1] * len(NT5), NT5[:4] if last else NT5)

def make_consts():
    cs = np.zeros((128, C_END), np.float32)
    cs[:, C_ID:C_ID + 128] = np.eye(128, dtype=np.float32)
    s = np.arange(128)[:, None]
    t = np.arange(128)[None, :]
    same = (s // GC) == (t // GC)
    cs[:, C_MF:C_MF + 128] = (same & (s <= t)).astype(np.float32)
    cs[:, C_MB:C_MB + 128] = (same & (s >= t)).astype(np.float32)
    rst = np.ones(NTOK, np.float32)
    rst[::GC] = 0.0
    cs[:, C_RST:C_END] = rst[None, :]
    return cs


def make_vecs(inp, b):
    v = np.zeros((128, D), np.float32)
    v[R_C] = inp["c"][b]
    v[R_CCTX] = inp["c_ctx"]
    v[R_ADAB:R_ADAB + 24] = inp["ada_b"].reshape(24, D)
    v[R_NMIX:R_NMIX + 4] = inp["norm_mix_g"]
    v[R_NFFN:R_NFFN + 4] = inp["norm_ffn_g"]
    v[R_FING] = inp["final_g"]
    v[R_PW1B:R_PW1B + 4] = inp["conv_pw1_b"].reshape(4, D)
    v[R_DWW:R_DWW + 62] = inp["conv_dw_w"].reshape(62, D)
    v[R_DWB:R_DWB + 2] = inp["conv_dw_b"]
    v[R_LNG:R_LNG + 2] = inp["conv_ln_g"]
    v[R_LNB:R_LNB + 2] = inp["conv_ln_b"]
    v[R_PW2B:R_PW2B + 2] = inp["conv_pw2_b"]
    v[R_LBL:R_LBL + 8] = inp["rec_lb_logits"].reshape(8, D)
    v[R_ONG:R_ONG + 2] = inp["rec_onorm_g"]
    return v


WKEYS = ["ada_w", "conv_pw1_w", "conv_pw2_w", "rec_w_in", "rec_w_o", "ffn_w1", "ffn_w3", "ffn_w2",
         "moe_router", "moe_w1", "moe_w3", "moe_w2"]


def make_in_maps(inp, cores, used=None):
    consts = make_consts()
    shared = {k: np.ascontiguousarray(inp[k], dtype=np.float32) for k in WKEYS if used is None or k in used}
    maps = []
    for b in cores:
        m = dict(shared)
        m["xin"] = np.ascontiguousarray(np.concatenate([inp["x"][b], inp["ctx"][b]], axis=0), dtype=np.float32)
        m["vecs"] = make_vecs(inp, b)
        m["consts"] = consts
        maps.append(m)
    return maps


_CACHE = {}


def kernel(**inputs):
    inp = {k: np.asarray(v) for k, v in inputs.items()}
    if "nc" not in _CACHE:
        _CACHE["prog"] = Prog()
        _CACHE["nc"] = _CACHE["prog"].build()
    nc = _CACHE["nc"]
    maps = make_in_maps(inp, list(range(8)), set(_CACHE["prog"].A.keys()))
    res = run_bass_kernel_spmd(nc, maps, core_ids=list(range(8)))
    out = np.stack([np.asarray(r["out"]) for r in res.results], axis=0)
    return out.astype(np.float32)
```
